# Optimizing a Trainium2 kernel written in Bass

```python
import math
import jax, jax.numpy as jnp
from jax import lax
import numpy as np

D_MODEL = 1024
BATCH = 1
SEQ = 16384
DEPTH = 2

N_BRANCHES = 4
BRANCH_WIDTH = 256
HEAD_DIM = 64
N_HEADS = BRANCH_WIDTH // HEAD_DIM
N_IDX_HEADS = 4
IDX_DIM = 32
TOPK_MAX = 256
CONV_WIDTH = 3
CHUNK = 128
N_GROUPS = 4
GROUP_DIM = BRANCH_WIDTH // N_GROUPS
Q_BLOCK = 128
PLE_DIM = 256
ROPE_THETA = 10000.0
EPS = 1e-6
IDX_W_SCALE = (N_IDX_HEADS * IDX_DIM) ** -0.5

IN_SPLITS = (
    BRANCH_WIDTH, BRANCH_WIDTH, BRANCH_WIDTH,
    N_IDX_HEADS * IDX_DIM, IDX_DIM, N_IDX_HEADS,
    BRANCH_WIDTH, BRANCH_WIDTH, BRANCH_WIDTH,
    BRANCH_WIDTH, BRANCH_WIDTH, BRANCH_WIDTH,
    BRANCH_WIDTH, BRANCH_WIDTH,
    N_BRANCHES * BRANCH_WIDTH,
    N_BRANCHES * D_MODEL,
)
IN_COLS = sum(IN_SPLITS)

kernel_name = "hybrid_dsa_conv_stickbreak_gmlp_block"


def _split_cols(h, sizes):
    out, off = [], 0
    for n in sizes:
        out.append(h[..., off:off + n])
        off += n
    return out


def rmsnorm(x, g):
    xf = x.astype(jnp.float32)
    y = xf * lax.rsqrt(jnp.mean(xf * xf, axis=-1, keepdims=True) + EPS)
    return (y * g.astype(jnp.float32)).astype(x.dtype)


def layernorm(x, g, b):
    xf = x.astype(jnp.float32)
    mu = jnp.mean(xf, axis=-1, keepdims=True)
    var = jnp.mean(jnp.square(xf - mu), axis=-1, keepdims=True)
    y = (xf - mu) * lax.rsqrt(var + EPS)
    return (y * g.astype(jnp.float32) + b.astype(jnp.float32)).astype(x.dtype)


def rope(x, pos):
    d = x.shape[-1]
    inv_freq = ROPE_THETA ** (-jnp.arange(0, d, 2, dtype=jnp.float32) / d)
    ang = pos.astype(jnp.float32)[..., None] * inv_freq
    cos = jnp.cos(ang)[:, :, None, :]
    sin = jnp.sin(ang)[:, :, None, :]
    xf = x.astype(jnp.float32)
    x1, x2 = xf[..., : d // 2], xf[..., d // 2:]
    return jnp.concatenate([x1 * cos - x2 * sin, x2 * cos + x1 * sin], axis=-1).astype(x.dtype)


def dsa_sparse_attention(q, k, v, iq, ik, iw):
    B, S, H, dh = q.shape
    topk = min(TOPK_MAX, S // 4)
    n_blk = S // Q_BLOCK
    key_pos = jnp.arange(S)
    neg = jnp.finfo(jnp.float32).min
    scale = HEAD_DIM ** -0.5

    def block(i):
        start = i * Q_BLOCK
        qb = lax.dynamic_slice_in_dim(q, start, Q_BLOCK, axis=1)
        iqb = lax.dynamic_slice_in_dim(iq, start, Q_BLOCK, axis=1)
        iwb = lax.dynamic_slice_in_dim(iw, start, Q_BLOCK, axis=1)
        q_pos = start + jnp.arange(Q_BLOCK)
        idx_logits = jnp.einsum('bqhd,bsd->bqhs', iqb, ik).astype(jnp.float32)
        score = jnp.einsum('bqh,bqhs->bqs', iwb.astype(jnp.float32) * IDX_W_SCALE,
                           jax.nn.relu(idx_logits))
        causal = key_pos[None, :] <= q_pos[:, None]
        score = jnp.where(causal[None], score, neg)
        _, sel = lax.top_k(score, topk)
        k_sel = jax.vmap(lambda kk, ii: kk[ii])(k, sel)
        v_sel = jax.vmap(lambda vv, ii: vv[ii])(v, sel)
        logits = jnp.einsum('bqhd,bqkhd->bhqk', qb, k_sel).astype(jnp.float32) * scale
        valid = sel <= q_pos[None, :, None]
        logits = jnp.where(valid[:, None], logits, neg)
        probs = jax.nn.softmax(logits, axis=-1)
        return jnp.einsum('bhqk,bqkhd->bqhd', probs.astype(v.dtype), v_sel)

    out = lax.map(block, jnp.arange(n_blk))
    return jnp.moveaxis(out, 0, 1).reshape(B, S, H * dh)


def short_gated_conv(gate_b, gate_c, x_in, conv_w, conv_b):
    W = x_in.shape[-1]
    y = gate_c * x_in
    conv = lax.conv_general_dilated(
        y, conv_w.astype(y.dtype)[:, None, :],
        window_strides=(1,), padding=[(CONV_WIDTH - 1, 0)],
        dimension_numbers=('NWC', 'WIO', 'NWC'), feature_group_count=W)
    return gate_b * (conv + conv_b)


def stick_breaking_attention(q, k, v):
    B, S, H, dh = q.shape
    n_blk = S // Q_BLOCK
    key_pos = jnp.arange(S)
    scale = HEAD_DIM ** -0.5

    def block(i):
        start = i * Q_BLOCK
        qb = lax.dynamic_slice_in_dim(q, start, Q_BLOCK, axis=1)
        q_pos = start + jnp.arange(Q_BLOCK)
        z = jnp.einsum('bqhd,bshd->bhqs', qb, k).astype(jnp.float32) * scale
        strict = (key_pos[None, :] < q_pos[:, None])[None, None]
        log_beta = jax.nn.log_sigmoid(z)
        log_keep = jnp.where(strict, jax.nn.log_sigmoid(-z), 0.0)
        after = lax.cumsum(log_keep, axis=3, reverse=True) - log_keep
        wts = jnp.where(strict, jnp.exp(log_beta + after), 0.0)
        return jnp.einsum('bhqs,bshd->bqhd', wts.astype(v.dtype), v)

    out = lax.map(block, jnp.arange(n_blk))
    return jnp.moveaxis(out, 0, 1).reshape(B, S, H * dh)


def chunked_spatial_gating(u, v, ln_g, ln_b, w_s, b_s):
    B, S, W = v.shape
    vn = layernorm(v, ln_g, ln_b).reshape(B, S // CHUNK, CHUNK, N_GROUPS, GROUP_DIM)
    mask = jnp.tril(jnp.ones((CHUNK, CHUNK), dtype=w_s.dtype))
    mixed = jnp.einsum('gts,bnsgd->bntgd', w_s * mask, vn) + b_s.T[None, None, :, :, None]
    return u * mixed.reshape(B, S, W)


def hybrid_layer(x, p_i, positions, g_pre, w_in, conv_w, conv_b, ln_g, ln_b,
                 w_spatial, b_spatial, b_merge, w_branch, w_out, g_post, w_ple, w_ple_gate):
    B, S, _ = x.shape
    h = rmsnorm(x, g_pre)
    proj = jnp.einsum('bsd,dc->bsc', h, w_in)
    (a_q, a_k, a_v, a_iq, a_ik, a_iw, b_b, b_c, b_x, c_q, c_k, c_v,
     d_u, d_v, gate_paths, merge_logits) = _split_cols(proj, IN_SPLITS)

    heads = lambda t: t.reshape(B, S, N_HEADS, HEAD_DIM)
    iq = rope(a_iq.reshape(B, S, N_IDX_HEADS, IDX_DIM), positions)
    ik = rope(a_ik[:, :, None, :], positions)[:, :, 0, :]
    o_a = dsa_sparse_attention(rope(heads(a_q), positions), rope(heads(a_k), positions),
                               heads(a_v), iq, ik, a_iw)
    o_b = short_gated_conv(b_b, b_c, b_x, conv_w, conv_b)
    o_c = stick_breaking_attention(heads(c_q), heads(c_k), heads(c_v))
    o_d = chunked_spatial_gating(d_u, d_v, ln_g, ln_b, w_spatial, b_spatial)

    o = jnp.stack([o_a, o_b, o_c, o_d], axis=2)
    o = o * jax.nn.silu(gate_paths.reshape(B, S, N_BRANCHES, BRANCH_WIDTH))
    z = jnp.einsum('bsnw,nwd->bsnd', o, w_branch)
    g = jax.nn.sigmoid((merge_logits + b_merge).reshape(B, S, N_BRANCHES, D_MODEL))
    merged = jnp.sum(g * z, axis=2)
    y = jnp.einsum('bsd,de->bse', merged, w_out)
    x = x + rmsnorm(y, g_post)
    ple = jnp.einsum('bsp,pd->bsd', p_i, w_ple)
    x = x + ple * jax.nn.sigmoid(jnp.einsum('bsd,de->bse', x, w_ple_gate))
    return x


def setup_inputs(seed: int = 0) -> dict:
    key = jax.random.key(seed)
    ks = jax.random.split(key, 20)
    nrm = lambda k, shape, s: jax.random.normal(k, shape, jnp.float32) * s
    W = BRANCH_WIDTH
    return {
        "x": nrm(ks[0], (BATCH, SEQ, D_MODEL), 1.0),
        "p": nrm(ks[1], (DEPTH, BATCH, SEQ, PLE_DIM), 1.0),
        "positions": jnp.broadcast_to(jnp.arange(SEQ, dtype=jnp.int32), (BATCH, SEQ)),
        "g_pre": 1.0 + nrm(ks[2], (DEPTH, D_MODEL), 0.02),
        "w_in": nrm(ks[3], (DEPTH, D_MODEL, IN_COLS), D_MODEL ** -0.5),
        "conv_w": nrm(ks[4], (DEPTH, CONV_WIDTH, W), CONV_WIDTH ** -0.5),
        "conv_b": nrm(ks[5], (DEPTH, W), 0.02),
        "ln_g": 1.0 + nrm(ks[6], (DEPTH, W), 0.02),
        "ln_b": nrm(ks[7], (DEPTH, W), 0.02),
        "w_spatial": nrm(ks[8], (DEPTH, N_GROUPS, CHUNK, CHUNK), CHUNK ** -0.5),
        "b_spatial": 1.0 + nrm(ks[9], (DEPTH, N_GROUPS, CHUNK), 0.02),
        "b_merge": nrm(ks[10], (DEPTH, N_BRANCHES * D_MODEL), 0.02),
        "w_branch": nrm(ks[11], (DEPTH, N_BRANCHES, W, D_MODEL), W ** -0.5),
        "w_out": nrm(ks[12], (DEPTH, D_MODEL, D_MODEL), D_MODEL ** -0.5),
        "g_post": 1.0 + nrm(ks[13], (DEPTH, D_MODEL), 0.02),
        "w_ple": nrm(ks[14], (DEPTH, PLE_DIM, D_MODEL), PLE_DIM ** -0.5),
        "w_ple_gate": nrm(ks[15], (DEPTH, D_MODEL, D_MODEL), D_MODEL ** -0.5),
    }


def reference(x, p, positions, g_pre, w_in, conv_w, conv_b, ln_g, ln_b, w_spatial, b_spatial,
              b_merge, w_branch, w_out, g_post, w_ple, w_ple_gate):
    for i in range(DEPTH):
        x = hybrid_layer(x, p[i], positions, g_pre[i], w_in[i], conv_w[i], conv_b[i], ln_g[i], ln_b[i],
                         w_spatial[i], b_spatial[i], b_merge[i], w_branch[i], w_out[i], g_post[i],
                         w_ple[i], w_ple_gate[i])
    return x
```

```python
import math
from contextlib import ExitStack

import numpy as np
import concourse.bass as bass
import concourse.mybir as mybir
from concourse.bass_utils import run_bass_kernel_spmd

F32 = mybir.dt.float32
BF16 = mybir.dt.bfloat16
I32 = mybir.dt.int32
AF = mybir.ActivationFunctionType
ALU = mybir.AluOpType
AX = mybir.AxisListType

NCORES = 8
S = 16384
D = 1024
NBLK = S // 128
LB = NBLK // NCORES
OWN = LB * 128
NG = 4
EPS = 1e-6
NIT = 22
TOPK = 256.0
IDX_W_SCALE = 128 ** -0.5
PI = math.pi

O_AQ, O_AK, O_AV, O_IQ, O_IK, O_IW = 0, 256, 512, 768, 896, 928
O_BB, O_BC, O_BX, O_CQ, O_CK, O_CV, O_DU, O_DV, O_GP, O_MG = 932, 1188, 1444, 1700, 1956, 2212, 2468, 2724, 2980, 4004


def _swap(off, n, hd):
    idx = []
    for h in range(n // hd):
        b = off + h * hd
        idx += list(range(b + hd // 2, b + hd)) + list(range(b, b + hd // 2))
    return idx


def _r(off, n):
    return list(range(off, off + n))


W1_COLS = _r(O_AK, 256) + _swap(O_AK, 256, 64) + _r(O_CK, 256) + _r(O_IK, 32) + _swap(O_IK, 32, 32) + _r(O_AV, 256) + _r(O_CV, 256)
WA_COLS = _r(O_AQ, 256) + _swap(O_AQ, 256, 64) + _r(O_CQ, 256) + _r(O_IQ, 128) + _swap(O_IQ, 128, 32)
WB_COLS = _r(O_BB, 256) + _r(O_BC, 256) + _r(O_BX, 256) + _r(O_DU, 256)
WC_COLS = _r(O_DV, 256) + _r(O_IW, 4)
WD_COLS = _r(O_GP, 1024)
WM_COLS = _r(O_MG, 4096)
C_W1 = 0
C_WA = C_W1 + len(W1_COLS)
C_WB = C_WA + len(WA_COLS)
C_WC = C_WB + len(WB_COLS)
C_WD = C_WC + len(WC_COLS)
C_WM = C_WD + len(WD_COLS)
NWCOL = C_WM + len(WM_COLS)
ALL_COLS = np.array(W1_COLS + WA_COLS + WB_COLS + WC_COLS + WD_COLS + WM_COLS, dtype=np.int64)

CO = {}
_off = 0
for _n, _w in [("ident", 128), ("tri", 128), ("ones", 128), ("trilst", 128), ("invfA", 1), ("sgnA", 1), ("nbA", 1),
               ("invfI", 1), ("sgnI", 1), ("nbI", 1), ("negpi", 1), ("one", 1), ("hw", NIT + 1), ("eg", 256), ("epsk", 1), ("eps", 1), ("pospi", 1)]:
    CO[_n] = (_off, _w)
    _off += _w
NCONST = _off


def make_consts():
    c = np.zeros((128, NCONST), np.float32)
    p = np.arange(128)
    c[:, CO["ident"][0]:CO["ident"][0] + 128] = np.eye(128)
    c[:, CO["tri"][0]:CO["tri"][0] + 128] = (p[:, None] >= p[None, :])
    c[:, CO["ones"][0]:CO["ones"][0] + 128] = 1.0
    c[:, CO["trilst"][0]:CO["trilst"][0] + 128] = (p[:, None] <= p[None, :])
    jA = p % 32
    c[:, CO["invfA"][0]] = (10000.0 ** (-(2.0 * jA) / 64.0)).astype(np.float32)
    sg = np.where((p % 64) < 32, -1.0, 1.0)
    c[:, CO["sgnA"][0]] = -sg
    c[:, CO["nbA"][0]] = PI * sg
    jI = p % 16
    c[:, CO["invfI"][0]] = (10000.0 ** (-(2.0 * jI) / 32.0)).astype(np.float32)
    sgi = np.where((p % 32) < 16, -1.0, 1.0)
    c[:, CO["sgnI"][0]] = -sgi
    c[:, CO["nbI"][0]] = PI * sgi
    c[:, CO["negpi"][0]] = -PI
    c[:, CO["pospi"][0]] = PI
    c[:, CO["one"][0]] = 1.0
    c[:, CO["epsk"][0]] = 1024.0 * EPS
    c[:, CO["eps"][0]] = EPS
    c[:, CO["hw"][0]:CO["hw"][0] + NIT + 1] = 2.0 ** (-(np.arange(NIT + 1) + 1.0))
    eg = np.zeros((128, 2, 128), np.float32)
    for g in range(4):
        eg[g, g // 2, (g % 2) * 64:(g % 2) * 64 + 64] = 1.0
    c[:, CO["eg"][0]:CO["eg"][0] + 256] = eg.reshape(128, 256)
    return c


def make_masks(core):
    t = np.arange(128)[:, None]
    s = np.arange(128)[None, :]
    cm = np.zeros((128, 8, 128), np.float32)
    ms = np.zeros((128, 8, 4, 128), np.float32)
    for o in range(8):
        if o < core:
            valid_le = np.ones((128, 128), bool)
            valid_lt = np.ones((128, 128), bool)
        elif o == core:
            valid_le = s <= t
            valid_lt = s < t
        else:
            valid_le = np.zeros((128, 128), bool)
            valid_lt = np.zeros((128, 128), bool)
        cm[:, o, :] = np.where(valid_le, 0.0, -1e30)
        ms[:, o, :, :] = valid_lt.T[:, None, :]
    return cm.reshape(128, 1024), np.ascontiguousarray(ms[:, :, 0, :]).reshape(128, 1024)


class Buf:
    __slots__ = ("w", "r")

    def __init__(self):
        self.w = None
        self.r = {}


class DSem:
    def __init__(self, K, name):
        self.K = K
        self.name = name
        self.h = K.newsem(name)
        self.cnt = 0

    def bump(self):
        if self.cnt >= 32000:
            self.h = self.K.newsem(self.name)
            self.cnt = 0
        self.cnt += 16
        return (self.h, self.cnt, None)


class Eng:
    def __init__(self, K, eng, name, sew=True):
        self.K, self.e, self.name, self.sew = K, eng, name, sew
        self.sem = K.newsem(name)
        self.cnt = 0
        self.waited = {}
        self.last = None

    def wait(self, tok):
        if tok is None:
            return
        sem, val, owner = tok
        if owner is self and not self.sew:
            return
        key = id(sem)
        if self.waited.get(key, 0) >= val:
            return
        self.e.wait_ge(sem, val)
        self.waited[key] = val

    def _deps(self, reads, writes):
        for b in reads:
            self.wait(b.w)
        for b in writes:
            self.wait(b.w)
            for own, t in b.r.items():
                if own is not self:
                    self.wait(t)

    def _mark(self, tok, reads, writes, owner):
        for b in reads:
            b.r[owner] = tok
        for b in writes:
            b.w = tok
            b.r = {}

    def op(self, fn, reads=(), writes=()):
        if self.K.mute:
            return None
        self._deps(reads, writes)
        if self.cnt >= 30000:
            self.sem = self.K.newsem(self.name)
            self.cnt = 0
        ins = fn(self.e)
        self.cnt += 1
        ins.then_inc(self.sem, 1)
        tok = (self.sem, self.cnt, self)
        self._mark(tok, reads, writes, self)
        self.last = tok
        return tok

    def dma(self, out, in_, dsem, reads=(), writes=()):
        if self.K.mute:
            return None
        self._deps(reads, writes)
        ins = self.e.dma_start(out=out, in_=in_)
        tok = dsem.bump()
        ins.then_inc(tok[0], 16)
        self._mark(tok, reads, writes, dsem)
        self.K.dsems[id(dsem)] = dsem
        return tok


class Kern:
    def __init__(self, nc, stack):
        self.nc, self.stack = nc, stack
        self._uid = 0
        self.mute = False
        self.dsems = {}
        self.pe = Eng(self, nc.tensor, "pe", sew=False)
        self.act = Eng(self, nc.scalar, "act")
        self.dve = Eng(self, nc.vector, "dve")
        self.pool = Eng(self, nc.gpsimd, "pool")
        self.sp = Eng(self, nc.sync, "sp")
        self.engs = [self.pe, self.act, self.dve, self.pool, self.sp]

    def newsem(self, name):
        self._uid += 1
        return self.stack.enter_context(self.nc.semaphore(f"{name}_{self._uid}"))

    def barrier(self):
        toks = [e.last for e in self.engs if e.last is not None]
        for d in self.dsems.values():
            if d.cnt:
                toks.append((d.h, d.cnt, None))
        for e in self.engs:
            for t in toks:
                if t[2] is not e:
                    e.wait(t)


import os
P1_CHUNKS = int(os.environ.get('P1_CHUNKS', S // 512))
NGROUPS = int(os.environ.get('NGROUPS', NG))
ASUB = int(os.environ.get('ASUB', 9))
CSUB = int(os.environ.get('CSUB', 99))
STAGES = os.environ.get('STAGES', 'gate,bd,d,q,score,bis,A,C,post').split(',')


def build_program():
    nc = bass.Bass("TRN2", target_bir_lowering=False)

    def din(name, shape, dt=F32):
        return nc.dram_tensor(name, list(shape), dt, kind="ExternalInput").ap()

    xT_full = din("xT_full", [D, S])
    xT_own = din("xT_own", [D, OWN])
    xT_halo = din("xT_halo", [D, 32])
    pT_own = din("pT_own", [256, OWN])
    pos_full = din("pos_full", [1, S], I32)
    pos_own = din("pos_own", [1, OWN], I32)
    w_ext = din("w_ext", [D, NWCOL])
    w_branch = din("w_branch", [4, 256, D])
    w_out = din("w_out", [D, D])
    w_ple = din("w_ple", [256, D])
    w_pg = din("w_pg", [D, D])
    wsT = din("wsT", [128, 4 * 128])
    vecs = din("vecs", [128, 64])
    lnb = din("lnb", [128, 512])
    bsp = din("bsp", [4, 128])
    consts = din("consts", [128, NCONST])
    cm_in = din("cm", [128, 1024])
    ms_in = din("ms", [128, 1024])
    out = nc.dram_tensor("xT_next", [D, OWN], F32, kind="ExternalOutput").ap()

    KTA = nc.dram_tensor("KTA", [2, 128, S], BF16)
    KTC = nc.dram_tensor("KTC", [2, 128, S], BF16)
    IKT = nc.dram_tensor("IKT", [32, S], BF16)
    VAC = nc.dram_tensor("VAC", [S, 512], BF16)

    with ExitStack() as stack:
        K = Kern(nc, stack)
        pe, act, dve, pool, sp = K.pe, K.act, K.dve, K.pool, K.sp

        def T(name, shape, dt=F32, st=stack):
            K._uid += 1
            return st.enter_context(nc.sbuf_tensor(f"{name}_{K._uid}", list(shape), dt))

        cst = T("cst", [128, NCONST])
        cstb = T("cstb", [128, 3 * 128 + 256], BF16)
        vec = T("vec", [128, 64])
        lnbt = T("lnbt", [128, 512])
        bspb = T("bspb", [4, 128], BF16)
        wst = T("wst", [128, 512], BF16)
        wsf = T("wsf", [128, 512])
        cmt = T("cmt", [128, 1024])
        mst = T("mst", [128, 1024])
        g32 = T("g32", [128, 16])
        yhalo = T("yhalo", [128, 2, 32])
        ps = [stack.enter_context(nc.psum_tensor(f"ps{i}", [128, 512], F32)) for i in range(8)]
        pb = [Buf() for _ in range(8)]
        b_cst, b_vec = Buf(), Buf()
        ld = DSem(K, "ld")

        def cc(name, a=0, n=None):
            o, w = CO[name]
            n = w if n is None else n
            return cst[:, o + a:o + a + n]

        identb = cstb[:, 0:128]
        trib = cstb[:, 128:256]
        onesb = cstb[:, 256:384]
        egb = cstb[:, 384:640]

        sp.dma(cst[:], consts, ld, writes=[b_cst])
        sp.dma(vec[:], vecs, ld, writes=[b_vec])
        sp.dma(lnbt[:], lnb, ld, writes=[b_vec])
        sp.dma(wsf[:], wsT, ld, writes=[b_vec])
        sp.dma(cmt[:], cm_in, ld, writes=[b_vec])
        sp.dma(mst[:], ms_in, ld, writes=[b_vec])
        pool.dma(bspb[:], bsp, ld, writes=[b_vec])
        dve.op(lambda e: e.tensor_copy(out=cstb[:, 0:384], in_=cst[:, 0:384]), reads=[b_cst], writes=[b_cst])
        dve.op(lambda e: e.tensor_copy(out=cstb[:, 384:640], in_=cc("eg")), reads=[b_cst], writes=[b_cst])
        for g in range(4):
            dve.op(lambda e, g=g: e.tensor_tensor(out=wst[:, g * 128:(g + 1) * 128], in0=wsf[:, g * 128:(g + 1) * 128],
                                                  in1=cc("trilst"), op=ALU.mult), reads=[b_vec, b_cst], writes=[b_vec])
        dve.op(lambda e: e.tensor_scalar(out=g32[:], in0=vec[:, 0:16], scalar1=32.0, scalar2=None, op0=ALU.mult),
               reads=[b_vec], writes=[b_vec])
        K.barrier()
        bmg = vec[:, 16:48]
        cw = vec[:, 48:54]
        cb = vec[:, 54:56]
        iota_dummy = None

        wbuf = [T(f"wbuf{i}", [128, 8, 1024], BF16) for i in range(2)]
        wb_b = [Buf(), Buf()]
        wsem = [DSem(K, "w0"), DSem(K, "w1")]
        wctr = [0]

        def load_w(src_ap_fn, nk, ncols):
            i = wctr[0] % 2
            wctr[0] += 1
            for k in range(nk):
                pool.dma(wbuf[i][:, k, 0:ncols], src_ap_fn(k), wsem[i], writes=[wb_b[i]] if k == 0 else [])
                if k > 0:
                    wb_b[i].w = (wsem[i].h, wsem[i].cnt, None)
            wb_b[i].w = (wsem[i].h, wsem[i].cnt, None)
            return wbuf[i], wb_b[i]

        def wext(c0, ncols):
            return lambda k: w_ext[k * 128:(k + 1) * 128, c0:c0 + ncols]

        def make_h(st, name, x_src_fn, ntok, xkeep=None, tst=None):
            tst = st if tst is None else tst
            hT = T(name + "_h", [128, 8, ntok], BF16, st)
            xt = T(name + "_x", [128, 8, ntok], F32, tst)
            sq = T(name + "_sq", [128, 8, ntok], BF16, tst)
            rr = T(name + "_r", [128, ntok], F32, tst)
            bx_, bsq, bh, br = Buf(), Buf(), Buf(), Buf()
            xs = DSem(K, name + "_xs")
            return dict(xt=xt, sq=sq, hT=hT, rr=rr, bx=bx_, bsq=bsq, bh=bh, br=br, xs=xs, ntok=ntok)

        def run_h(hh, x_src_fn, pbank):
            ntok = hh["ntok"]
            xt, sq, hT, rr = hh["xt"], hh["sq"], hh["hT"], hh["rr"]
            for k in range(8):
                sp.dma(xt[:, k, :], x_src_fn(k), hh["xs"], writes=[hh["bx"]] if k == 0 else [])
            hh["bx"].w = (hh["xs"].h, hh["xs"].cnt, None)
            act.op(lambda e: e.activation(out=sq[:], in_=xt[:], func=AF.Square), reads=[hh["bx"]], writes=[hh["bsq"]])
            for k in range(8):
                pe.op(lambda e, k=k: e.matmul(ps[pbank][:, 0:ntok], lhsT=onesb, rhs=sq[:, k, :], start=(k == 0), stop=(k == 7)),
                      reads=[hh["bsq"], b_cst], writes=[pb[pbank]])
            act.op(lambda e: e.activation(out=rr[:], in_=ps[pbank][:, 0:ntok], func=AF.Ln, bias=cc("epsk"), scale=1.0), reads=[pb[pbank], b_cst], writes=[hh["br"]])
            act.op(lambda e: e.activation(out=rr[:], in_=rr[:], func=AF.Exp, scale=-0.5), reads=[hh["br"]], writes=[hh["br"]])
            for k in range(8):
                dve.op(lambda e, k=k: e.scalar_tensor_tensor(out=hT[:, k, :], in0=xt[:, k, :], scalar=g32[:, k:k + 1], in1=rr[:],
                                                              op0=ALU.mult, op1=ALU.mult),
                        reads=[hh["bx"], hh["br"], b_vec], writes=[hh["bh"]])

        def rope_tables(st, name, pos_src, ntok, npart, invf, sgn, nb):
            pi_ = T(name + "_pi", [npart, ntok], I32, st)
            pf = T(name + "_pf", [npart, ntok], F32, st)
            a1 = T(name + "_a1", [npart, ntok], F32, st)
            cos = T(name + "_cos", [npart, ntok], F32, st)
            sin = T(name + "_sin", [npart, ntok], F32, st)
            return dict(pi=pi_, pf=pf, a1=a1, cos=cos, sin=sin, b=Buf(), bt=Buf(), ds=DSem(K, name + "_d"),
                        npart=npart, invf=invf, sgn=sgn, nb=nb, ntok=ntok)

        def run_rope_tables(rt, pos_src):
            n = rt["npart"]
            sp.dma(rt["pi"][:], pos_src.partition_broadcast(n), rt["ds"], writes=[rt["b"]])
            dve.op(lambda e: e.tensor_copy(out=rt["pf"][:], in_=rt["pi"][:]), reads=[rt["b"]], writes=[rt["b"]])
            C1 = 6.28125
            C2 = 2 * PI - C1
            pf, a1, pi_, cos, sin = rt["pf"], rt["a1"], rt["pi"], rt["cos"], rt["sin"]
            B = [rt["b"]]
            dve.op(lambda e: e.tensor_scalar(out=a1[:], in0=pf[:], scalar1=rt["invf"][0:n, :], scalar2=None, op0=ALU.mult), reads=B + [b_cst], writes=B)
            dve.op(lambda e: e.tensor_scalar(out=pi_[:], in0=a1[:], scalar1=1.0 / (2 * PI), scalar2=None, op0=ALU.mult), reads=B, writes=B)
            dve.op(lambda e: e.tensor_copy(out=pf[:], in_=pi_[:]), reads=B, writes=B)
            dve.op(lambda e: e.scalar_tensor_tensor(out=a1[:], in0=pf[:], scalar=-C1, in1=a1[:], op0=ALU.mult, op1=ALU.add), reads=B, writes=B)
            dve.op(lambda e: e.scalar_tensor_tensor(out=a1[:], in0=pf[:], scalar=-C2, in1=a1[:], op0=ALU.mult, op1=ALU.add), reads=B, writes=B)
            dve.op(lambda e: e.tensor_scalar(out=pf[:], in0=a1[:], scalar1=0.0, scalar2=2 * PI, op0=ALU.is_lt, op1=ALU.mult), reads=B, writes=B)
            dve.op(lambda e: e.tensor_tensor(out=a1[:], in0=a1[:], in1=pf[:], op=ALU.add), reads=B, writes=B)
            dve.op(lambda e: e.tensor_scalar(out=a1[:], in0=a1[:], scalar1=0.0, scalar2=2 * PI, op0=ALU.max, op1=ALU.min), reads=B, writes=B)
            act.op(lambda e: e.activation(out=sin[:], in_=a1[:], func=AF.Sin, bias=rt["nb"][0:n, :], scale=rt["sgn"][0:n, :]),
                   reads=B + [b_cst], writes=[rt["bt"]])
            dve.op(lambda e: e.tensor_scalar(out=pf[:], in0=a1[:], scalar1=0.5 * PI, scalar2=None, op0=ALU.add), reads=B, writes=B)
            dve.op(lambda e: e.tensor_scalar(out=cos[:], in0=pf[:], scalar1=2 * PI, scalar2=-2 * PI, op0=ALU.is_ge, op1=ALU.mult), reads=B + [rt["bt"]], writes=B)
            dve.op(lambda e: e.tensor_tensor(out=pf[:], in0=pf[:], in1=cos[:], op=ALU.add), reads=B, writes=B)
            dve.op(lambda e: e.tensor_scalar(out=pf[:], in0=pf[:], scalar1=0.0, scalar2=2 * PI, op0=ALU.max, op1=ALU.min), reads=B, writes=B)
            act.op(lambda e: e.activation(out=cos[:], in_=pf[:], func=AF.Sin, bias=cc("pospi")[0:n, :], scale=-1.0),
                   reads=B + [b_cst], writes=[rt["bt"]])

        def proj_fm(w, wbk, c0, m, hT, bh, ntok, pbank, tsl=None):
            for k in range(8):
                rhs = hT[:, k, :] if tsl is None else hT[:, k, tsl]
                pe.op(lambda e, k=k, rhs=rhs: e.matmul(ps[pbank][0:m, 0:ntok], lhsT=w[:, k, c0:c0 + m], rhs=rhs,
                                                         start=(k == 0), stop=(k == 7)),
                      reads=[wbk, bh], writes=[pb[pbank]])

        def rope_evac(pb_x, pb_s, m, ntok, rt, tmp, btmp, dst, bdst):
            dve.op(lambda e: e.tensor_tensor(out=tmp[0:m, 0:ntok], in0=ps[pb_x][0:m, 0:ntok], in1=rt["cos"][0:m, :], op=ALU.mult),
                   reads=[pb[pb_x], rt["bt"]], writes=[btmp])
            dve.op(lambda e: e.tensor_tensor(out=tmp[0:m, 512:512 + ntok], in0=ps[pb_s][0:m, 0:ntok], in1=rt["sin"][0:m, :], op=ALU.mult),
                   reads=[pb[pb_s], rt["bt"]], writes=[btmp])
            pool.op(lambda e: e.tensor_tensor(out=dst, in0=tmp[0:m, 0:ntok], in1=tmp[0:m, 512:512 + ntok], op=ALU.add),
                    reads=[btmp], writes=[bdst])

        b_dram = Buf()
        with ExitStack() as st1:
            w1, w1b = load_w(wext(C_W1, 1024), 8, 1024)
            w1v, w1vb = load_w(wext(C_W1 + 832, 512), 8, 512)
            hh = [make_h(st1, f"p1h{i}", None, 512) for i in range(2)]
            rtA = [rope_tables(st1, "p1ra", None, 512, 128, cc("invfA"), cc("sgnA"), cc("nbA"))] * 2
            rtI = [rope_tables(st1, "p1ri", None, 512, 32, cc("invfI"), cc("sgnI"), cc("nbI"))] * 2
            tmp = T("p1tmp", [128, 1024], F32, st1)
            btmp = Buf()
            kst = [T(f"p1k{i}", [128, 5, 512], BF16, st1) for i in range(2)]
            bk = [Buf(), Buf()]
            vst = [T(f"p1v{i}", [128, 4, 512], BF16, st1) for i in range(2)]
            bv = [Buf(), Buf()]
            osem = [DSem(K, "p1o0"), DSem(K, "p1o1")]
            for ch in range(P1_CHUNKS):
                i = ch % 2
                t0 = ch * 512
                run_h(hh[i], lambda k, t0=t0: xT_full[k * 128:(k + 1) * 128, t0:t0 + 512], 7)
                run_rope_tables(rtA[i], pos_full[:, t0:t0 + 512])
                run_rope_tables(rtI[i], pos_full[:, t0:t0 + 512])
                hT, bh = hh[i]["hT"], hh[i]["bh"]
                for pr in range(2):
                    proj_fm(w1, w1b, pr * 128, 128, hT, bh, 512, 0)
                    proj_fm(w1, w1b, 256 + pr * 128, 128, hT, bh, 512, 1)
                    rope_evac(0, 1, 128, 512, rtA[i], tmp, btmp, kst[i][:, pr, :], bk[i])
                for pr in range(2):
                    proj_fm(w1, w1b, 512 + pr * 128, 128, hT, bh, 512, 2 + pr)
                    act.op(lambda e, pr=pr: e.copy(out=kst[i][:, 2 + pr, :], in_=ps[2 + pr][:, :]), reads=[pb[2 + pr]], writes=[bk[i]])
                proj_fm(w1, w1b, 768, 32, hT, bh, 512, 0)
                proj_fm(w1, w1b, 800, 32, hT, bh, 512, 1)
                rope_evac(0, 1, 32, 512, rtI[i], tmp, btmp, kst[i][0:32, 4, :], bk[i])
                for bl in range(4):
                    pbk = 4 + (bl % 2)
                    for k in range(8):
                        pe.op(lambda e, k=k, bl=bl, pbk=pbk: e.matmul(ps[pbk][:, :], lhsT=hT[:, k, bl * 128:(bl + 1) * 128], rhs=w1v[:, k, 0:512],
                                                                       start=(k == 0), stop=(k == 7)), reads=[bh, w1vb], writes=[pb[pbk]])
                    act.op(lambda e, bl=bl, pbk=pbk: e.copy(out=vst[i][:, bl, :], in_=ps[pbk][:, :]), reads=[pb[pbk]], writes=[bv[i]])
                for pr in range(2):
                    sp.dma(KTA[pr, :, t0:t0 + 512], kst[i][:, pr, :], osem[i], reads=[bk[i]] if pr == 0 else [], writes=[b_dram] if pr == 0 else [])
                    sp.dma(KTC[pr, :, t0:t0 + 512], kst[i][:, 2 + pr, :], osem[i])
                sp.dma(IKT[:, t0:t0 + 512], kst[i][0:32, 4, :], osem[i])
                sp.dma(VAC[t0:t0 + 512, :].rearrange("(b p) c -> p b c", p=128), vst[i][:], osem[i], reads=[bv[i]])
                tk = (osem[i].h, osem[i].cnt, None)
                bk[i].r[osem[i]] = tk
                bv[i].r[osem[i]] = tk
                b_dram.w = tk
            wB, wBb = load_w(wext(C_WB, 1024), 8, 1024)
            hhal = make_h(st1, "hal", None, 32)
            run_h(hhal, lambda k: xT_halo[k * 128:(k + 1) * 128, :], 7)
            bhal = Buf()
            for tl in range(2):
                proj_fm(wB, wBb, 256 + tl * 128, 128, hhal["hT"], hhal["bh"], 32, 0)
                proj_fm(wB, wBb, 512 + tl * 128, 128, hhal["hT"], hhal["bh"], 32, 1)
                act.op(lambda e: e.copy(out=tmp[:, 0:32], in_=ps[0][:, 0:32]), reads=[pb[0]], writes=[btmp])
                dve.op(lambda e, tl=tl: e.tensor_tensor(out=yhalo[:, tl, :], in0=ps[1][:, 0:32], in1=tmp[:, 0:32], op=ALU.mult),
                       reads=[pb[1], btmp], writes=[bhal])
            K.barrier()

        kvs = [DSem(K, f"kv{i}") for i in range(3)]
        kt = [T(f"kt{i}", [128, 2, 512], BF16) for i in range(3)]
        vt = [T(f"vt{i}", [128, 4, 256], BF16) for i in range(3)]
        ikt = [T(f"ikt{i}", [32, 512], BF16) for i in range(3)]
        kvb = [Buf() for _ in range(3)]
        kvctr = [0]

        def load_kv(which, c0):
            i = kvctr[0] % 3
            kvctr[0] += 1
            if which == "I":
                sp.dma(ikt[i][:], IKT[:, c0:c0 + 512], kvs[i], writes=[kvb[i]])
            else:
                KT = KTA if which == "A" else KTC
                vo = 0 if which == "A" else 256
                sp.dma(kt[i][:], KT[:, :, c0:c0 + 512].rearrange("a p t -> p a t"), kvs[i], writes=[kvb[i]])
                sp.dma(vt[i][:], VAC[c0:c0 + 512, vo:vo + 256].rearrange("(b p) c -> p b c", p=128), kvs[i])
                kvb[i].w = (kvs[i].h, kvs[i].cnt, None)
            return i

        osem2 = DSem(K, "out")
        b_out = Buf()
        for gi in range(NGROUPS):
            tg0 = gi * 512
            K.mute = False
            with ExitStack() as sg:
                with ExitStack() as sth:
                    hg = make_h(sg, "hg", None, 512, tst=sth)
                    run_h(hg, lambda k: xT_own[k * 128:(k + 1) * 128, tg0:tg0 + 512], 7)
                    K.barrier()
                hT, bh = hg["hT"], hg["bh"]
                oT = T("oT", [128, 4, 2, 512], BF16, sg)
                boT = Buf()
                gsl = T("gsl", [128, 8, 512], BF16, sg)
                bgs = Buf()
                pTb = T("pTb", [128, 2, 512], BF16, sg)
                bpT = Buf()
                dpt = DSem(K, "dpt")
                for k in range(2):
                    pool.dma(pTb[:, k, :], pT_own[k * 128:(k + 1) * 128, tg0:tg0 + 512], dpt, writes=[bpT] if k == 0 else [])
                bpT.w = (dpt.h, dpt.cnt, None)
                qA = T("qA", [128, 2, 512], BF16, sg)
                qC = T("qC", [128, 2, 512], BF16, sg)
                qAz = T("qAz", [128, 4, 512], BF16, sg)
                qCz = T("qCz", [128, 4, 512], BF16, sg)
                iq = T("iq", [32, 4, 512], BF16, sg)
                wq = T("wq", [128, 4, 4], F32, sg)
                bq = Buf()
                K.mute = ("gate" not in STAGES)
                wD, wDb = load_w(wext(C_WD, 1024), 8, 1024)
                for tl in range(8):
                    pbk = tl % 2
                    proj_fm(wD, wDb, tl * 128, 128, hT, bh, 512, pbk)
                    act.op(lambda e, tl=tl, pbk=pbk: e.activation(out=gsl[:, tl, :], in_=ps[pbk][:, :], func=AF.Silu),
                           reads=[pb[pbk]], writes=[bgs])
                K.mute = ("bd" not in STAGES)
                with ExitStack() as sbd:
                    wB, wBb = load_w(wext(C_WB, 1024), 8, 1024)
                    bbt = T("bbt", [128, 2, 512], F32, sbd)
                    ypad = T("ypad", [128, 2, 4, 130], F32, sbd)
                    acc = T("acc", [128, 2, 512], F32, sbd)
                    ut = T("ut", [128, 2, 512], F32, sbd)
                    bbd = Buf()
                    tmpb = T("tmpb", [128, 512], F32, sbd)
                    btb = Buf()
                    for tl in range(2):
                        proj_fm(wB, wBb, tl * 128, 128, hT, bh, 512, 0)
                        act.op(lambda e, tl=tl: e.copy(out=bbt[:, tl, :], in_=ps[0][:, :]), reads=[pb[0]], writes=[bbd])
                        proj_fm(wB, wBb, 256 + tl * 128, 128, hT, bh, 512, 1)
                        proj_fm(wB, wBb, 512 + tl * 128, 128, hT, bh, 512, 2)
                        act.op(lambda e: e.copy(out=tmpb[:], in_=ps[1][:, :]), reads=[pb[1]], writes=[btb])
                        dve.op(lambda e, tl=tl: e.tensor_tensor(out=ypad[:, tl, :, 2:130], in0=ps[2][:, :].rearrange("p (b t) -> p b t", b=4),
                                                                in1=tmpb[:].rearrange("p (b t) -> p b t", b=4), op=ALU.mult),
                               reads=[pb[2], btb], writes=[bbd])
                        pool.op(lambda e, tl=tl: e.tensor_copy(out=ypad[:, tl, :, 0:2],
                                                               in_=yhalo[:, tl, gi * 8:gi * 8 + 8].rearrange("p (b t) -> p b t", b=4)),
                                reads=[bhal], writes=[bbd])
                        proj_fm(wB, wBb, 768 + tl * 128, 128, hT, bh, 512, 3)
                        act.op(lambda e, tl=tl: e.copy(out=ut[:, tl, :], in_=ps[3][:, :]), reads=[pb[3]], writes=[bbd])
                    for tl in range(2):
                        a3 = acc[:, tl, :].rearrange("p (b t) -> p b t", b=4)
                        pool.op(lambda e, tl=tl, a3=a3: e.tensor_scalar(out=a3, in0=ypad[:, tl, :, 0:128], scalar1=cw[:, tl * 3:tl * 3 + 1],
                                                                         scalar2=None, op0=ALU.mult), reads=[bbd, b_vec], writes=[bbd])
                        for kk in (1, 2):
                            dve.op(lambda e, tl=tl, a3=a3, kk=kk: e.scalar_tensor_tensor(out=a3, in0=ypad[:, tl, :, kk:kk + 128],
                                                                                         scalar=cw[:, tl * 3 + kk:tl * 3 + kk + 1], in1=a3,
                                                                                         op0=ALU.mult, op1=ALU.add), reads=[bbd, b_vec], writes=[bbd])
                        dve.op(lambda e, tl=tl: e.scalar_tensor_tensor(out=acc[:, tl, :], in0=acc[:, tl, :], scalar=cb[:, tl:tl + 1], in1=bbt[:, tl, :],
                                                                        op0=ALU.add, op1=ALU.mult), reads=[bbd, b_vec], writes=[bbd])
                        pool.op(lambda e, tl=tl: e.tensor_tensor(out=oT[:, 1, tl, :], in0=acc[:, tl, :], in1=gsl[:, 2 + tl, :], op=ALU.mult),
                                reads=[bbd, bgs], writes=[boT])
                    K.mute = ("d" not in STAGES)
                    wC_, wCb = load_w(wext(C_WC, 260), 8, 260)
                    vn = T("vn", [128, 256], F32, sbd)
                    vnz = T("vnz", [128, 4, 128], BF16, sbd)
                    st4 = T("st4", [128, 8], F32, sbd)
                    junk = T("junkd", [128, 256], F32, sbd)
                    bvn, bst = Buf(), Buf()
                    pool.op(lambda e: e.memset(vnz[:], 0.0), writes=[bvn])
                    for bl in range(4):
                        tsl = slice(bl * 128, (bl + 1) * 128)
                        for k in range(8):
                            pe.op(lambda e, k=k, tsl=tsl: e.matmul(ps[4][:, 0:260], lhsT=hT[:, k, tsl], rhs=wC_[:, k, 0:260], start=(k == 0), stop=(k == 7)),
                                  reads=[bh, wCb], writes=[pb[4]])
                        dve.op(lambda e, bl=bl: e.tensor_scalar(out=wq[:, bl, :], in0=ps[4][:, 256:260], scalar1=IDX_W_SCALE, scalar2=None, op0=ALU.mult),
                               reads=[pb[4]], writes=[bq])
                        act.op(lambda e: e.activation(out=vn[:], in_=ps[4][:, 0:256], func=AF.Copy, accum_out=st4[:, 0:1]), reads=[pb[4]], writes=[bvn, bst])
                        act.op(lambda e: e.activation(out=junk[:], in_=ps[4][:, 0:256], func=AF.Square, accum_out=st4[:, 1:2]), reads=[pb[4]], writes=[bst])
                        dve.op(lambda e: e.tensor_scalar(out=st4[:, 2:3], in0=st4[:, 0:1], scalar1=1.0 / 256, scalar2=None, op0=ALU.mult), reads=[bst], writes=[bst])
                        dve.op(lambda e: e.tensor_tensor(out=st4[:, 3:4], in0=st4[:, 2:3], in1=st4[:, 2:3], op=ALU.mult), reads=[bst], writes=[bst])
                        dve.op(lambda e: e.scalar_tensor_tensor(out=st4[:, 4:5], in0=st4[:, 1:2], scalar=1.0 / 256, in1=st4[:, 3:4], op0=ALU.mult, op1=ALU.subtract),
                               reads=[bst], writes=[bst])
                        act.op(lambda e: e.activation(out=st4[:, 5:6], in_=st4[:, 4:5], func=AF.Ln, bias=cc("eps"), scale=1.0), reads=[bst, b_cst], writes=[bst])
                        act.op(lambda e: e.activation(out=st4[:, 5:6], in_=st4[:, 5:6], func=AF.Exp, scale=-0.5), reads=[bst], writes=[bst])
                        dve.op(lambda e: e.tensor_scalar(out=vn[:], in0=vn[:], scalar1=st4[:, 2:3], scalar2=st4[:, 5:6], op0=ALU.subtract, op1=ALU.mult),
                               reads=[bvn, bst], writes=[bvn])
                        dve.op(lambda e: e.tensor_tensor(out=vn[:], in0=vn[:], in1=lnbt[:, 0:256], op=ALU.mult), reads=[bvn, b_vec], writes=[bvn])
                        for g in range(4):
                            dve.op(lambda e, g=g: e.tensor_tensor(out=vnz[:, g, (g % 2) * 64:(g % 2) * 64 + 64], in0=vn[:, g * 64:(g + 1) * 64],
                                                                  in1=lnbt[:, 256 + g * 64:256 + (g + 1) * 64], op=ALU.add), reads=[bvn, b_vec], writes=[bvn])
                        for pr in range(2):
                            for gg in range(2):
                                g = 2 * pr + gg
                                pe.op(lambda e, g=g, gg=gg: e.matmul(ps[5][:, 0:128], lhsT=vnz[:, g, :], rhs=wst[:, g * 128:(g + 1) * 128], start=(gg == 0), stop=False),
                                      reads=[bvn, b_vec], writes=[pb[5]])
                            pe.op(lambda e, pr=pr: e.matmul(ps[5][:, 0:128], lhsT=egb[0:4, pr * 128:(pr + 1) * 128], rhs=bspb[0:4, :], start=False, stop=True),
                                  reads=[b_cst, b_vec], writes=[pb[5]])
                            dve.op(lambda e, pr=pr, tsl=tsl: e.tensor_tensor(out=ut[:, pr, tsl], in0=ps[5][:, 0:128], in1=ut[:, pr, tsl], op=ALU.mult),
                                   reads=[pb[5], bbd], writes=[bbd])
                            pool.op(lambda e, pr=pr, tsl=tsl: e.tensor_tensor(out=oT[:, 3, pr, tsl], in0=ut[:, pr, tsl], in1=gsl[:, 6 + pr, tsl], op=ALU.mult),
                                    reads=[bbd, bgs], writes=[boT])
                    K.barrier()

                K.mute = ("q" not in STAGES)
                with ExitStack() as sat:
                    rtA = rope_tables(sat, "rqa", None, 512, 128, cc("invfA"), cc("sgnA"), cc("nbA"))
                    rtI = rope_tables(sat, "rqi", None, 512, 32, cc("invfI"), cc("sgnI"), cc("nbI"))
                    run_rope_tables(rtA, pos_own[:, tg0:tg0 + 512])
                    run_rope_tables(rtI, pos_own[:, tg0:tg0 + 512])
                    tmp = T("tmpq", [128, 1024], F32, sat)
                    btmp = Buf()
                    wA, wAb = load_w(wext(C_WA, 1024), 8, 1024)
                    pool.op(lambda e: e.memset(qAz[:], 0.0), writes=[bq])
                    pool.op(lambda e: e.memset(qCz[:], 0.0), writes=[bq])
                    for pr in range(2):
                        proj_fm(wA, wAb, pr * 128, 128, hT, bh, 512, 0)
                        proj_fm(wA, wAb, 256 + pr * 128, 128, hT, bh, 512, 1)
                        rope_evac(0, 1, 128, 512, rtA, tmp, btmp, qA[:, pr, :], bq)
                        pool.op(lambda e, pr=pr: e.tensor_copy(out=qAz[0:64, 2 * pr, :], in_=qA[0:64, pr, :]), reads=[bq], writes=[bq])
                        pool.op(lambda e, pr=pr: e.tensor_copy(out=qAz[64:128, 2 * pr + 1, :], in_=qA[64:128, pr, :]), reads=[bq], writes=[bq])
                        proj_fm(wA, wAb, 512 + pr * 128, 128, hT, bh, 512, 2)
                        act.op(lambda e, pr=pr: e.copy(out=qC[:, pr, :], in_=ps[2][:, :]), reads=[pb[2]], writes=[bq])
                        pool.op(lambda e, pr=pr: e.tensor_copy(out=qCz[0:64, 2 * pr, :], in_=qC[0:64, pr, :]), reads=[bq], writes=[bq])
                        pool.op(lambda e, pr=pr: e.tensor_copy(out=qCz[64:128, 2 * pr + 1, :], in_=qC[64:128, pr, :]), reads=[bq], writes=[bq])
                    for h in range(4):
                        proj_fm(wA, wAb, 768 + h * 32, 32, hT, bh, 512, 0)
                        proj_fm(wA, wAb, 896 + h * 32, 32, hT, bh, 512, 1)
                        rope_evac(0, 1, 32, 512, rtI, tmp, btmp, iq[0:32, h, :], bq)
                    K.barrier()

                with ExitStack() as sa:
                    Sc = T("Sc", [128, S], F32, sa)
                    bS = Buf()
                    junk = T("junk", [128, 2048], BF16, sa)
                    rl = [T(f"rl{i}", [128, 512], F32, sa) for i in range(2)]
                    brl = [Buf(), Buf()]
                    bis = T("bis", [128, 64], F32, sa)
                    bbis = Buf()
                    Et = [T(f"Et{i}", [128, 512], F32, sa) for i in range(2)]
                    Lt = [T(f"Lt{i}", [128, 512], BF16, sa) for i in range(2)]
                    Xt = [T(f"Xt{i}", [128, 512], F32, sa) for i in range(2)]
                    Wt = [T(f"Wt{i}", [128, 512], BF16, sa) for i in range(2)]
                    bE = [Buf(), Buf()]
                    bL = [Buf(), Buf()]
                    bX = [Buf(), Buf()]
                    bW = [Buf(), Buf()]
                    car = T("car", [32, 1024], BF16, sa)
                    bcar = Buf()
                    fin = T("fin", [128, 512], F32, sa)
                    bfin = Buf()
                    identf = cc("ident")

                    def p1(bl):
                        l = gi * 4 + bl
                        NK = (8 * l + 8) * 128
                        nkb = 8 * l + 8
                        tsl = slice(bl * 128, (bl + 1) * 128)
                        K.mute = ("score" not in STAGES)
                        for c0 in range(0, NK, 512):
                            ki = load_kv("I", c0)
                            for h in range(4):
                                pbk = h % 2
                                pe.op(lambda e, h=h, pbk=pbk, ki=ki: e.matmul(ps[pbk][:, :], lhsT=iq[0:32, h, tsl], rhs=ikt[ki][0:32, :], start=True, stop=True),
                                      reads=[bq, kvb[ki]], writes=[pb[pbk]])
                                act.op(lambda e, pbk=pbk: e.activation(out=rl[pbk][:], in_=ps[pbk][:, :], func=AF.Relu), reads=[pb[pbk]], writes=[brl[pbk]])
                                if h == 0:
                                    dve.op(lambda e, c0=c0, pbk=pbk: e.tensor_scalar(out=Sc[:, c0:c0 + 512], in0=rl[pbk][:], scalar1=wq[:, bl, 0:1], scalar2=None, op0=ALU.mult),
                                           reads=[brl[pbk], bq], writes=[bS])
                                else:
                                    dve.op(lambda e, c0=c0, pbk=pbk, h=h: e.scalar_tensor_tensor(out=Sc[:, c0:c0 + 512], in0=rl[pbk][:], scalar=wq[:, bl, h:h + 1],
                                                                                                 in1=Sc[:, c0:c0 + 512], op0=ALU.mult, op1=ALU.add),
                                           reads=[brl[pbk], bq], writes=[bS])
                        K.mute = ("bis" not in STAGES)
                        tail = Sc[:, NK - 1024:NK]
                        mn, mx, mn2, lo, w0, cnt, dd, mid = (bis[:, i:i + 1] for i in range(8))
                        hwt = bis[:, 8:8 + NIT + 1]
                        cnts = bis[:, 40:48]
                        dve.op(lambda e: e.tensor_tensor(out=fin[:, 0:512], in0=tail[:, 0:512], in1=cmt[:, 0:512], op=ALU.subtract), reads=[bS, b_vec], writes=[bfin])
                        dve.op(lambda e: e.tensor_reduce(out=mn, in_=fin[:, 0:512], axis=AX.X, op=ALU.min), reads=[bfin], writes=[bbis])
                        dve.op(lambda e: e.tensor_tensor(out=fin[:, 0:512], in0=tail[:, 512:1024], in1=cmt[:, 512:1024], op=ALU.subtract), reads=[bS, b_vec, bbis], writes=[bfin])
                        dve.op(lambda e: e.tensor_reduce(out=mn2, in_=fin[:, 0:512], axis=AX.X, op=ALU.min), reads=[bfin], writes=[bbis])
                        dve.op(lambda e: e.tensor_tensor(out=mn, in0=mn, in1=mn2, op=ALU.min), reads=[bbis], writes=[bbis])
                        if NK > 1024:
                            dve.op(lambda e: e.tensor_reduce(out=mn2, in_=Sc[:, 0:NK - 1024], axis=AX.X, op=ALU.min), reads=[bS, bbis], writes=[bbis])
                            dve.op(lambda e: e.tensor_tensor(out=mn, in0=mn, in1=mn2, op=ALU.min), reads=[bbis], writes=[bbis])
                        dve.op(lambda e: e.tensor_tensor(out=tail, in0=tail, in1=cmt[:], op=ALU.add), reads=[bS, b_vec], writes=[bS])
                        dve.op(lambda e: e.tensor_reduce(out=mx, in_=Sc[:, 0:NK], axis=AX.X, op=ALU.max), reads=[bS, bbis], writes=[bbis])
                        dve.op(lambda e: e.tensor_tensor(out=w0, in0=mx, in1=mn, op=ALU.subtract), reads=[bbis], writes=[bbis])
                        dve.op(lambda e: e.tensor_scalar(out=w0, in0=w0, scalar1=1.001, scalar2=1e-20, op0=ALU.mult, op1=ALU.add), reads=[bbis], writes=[bbis])
                        dve.op(lambda e: e.tensor_scalar(out=hwt, in0=cc("hw"), scalar1=w0, scalar2=None, op0=ALU.mult), reads=[bbis, b_cst], writes=[bbis])
                        dve.op(lambda e: e.tensor_tensor(out=mid, in0=mn, in1=hwt[:, 0:1], op=ALU.add), reads=[bbis], writes=[bbis])
                        dve.op(lambda e: e.tensor_copy(out=lo, in_=mn), reads=[bbis], writes=[bbis])
                        nch = (NK + 2047) // 2048
                        for it in range(NIT):
                            for ci in range(nch):
                                a = ci * 2048
                                wdt = min(2048, NK - a)
                                dve.op(lambda e, a=a, wdt=wdt, ci=ci: e.tensor_scalar(out=junk[:, 0:wdt], in0=Sc[:, a:a + wdt], scalar1=mid, scalar2=0.0,
                                                                                      op0=ALU.is_ge, op1=ALU.add, accum_out=cnts[:, ci:ci + 1]),
                                       reads=[bS, bbis] if ci == 0 else [bS], writes=[])
                            bbis.w = dve.last
                            if nch > 1:
                                dve.op(lambda e: e.tensor_reduce(out=cnt, in_=cnts[:, 0:nch], axis=AX.X, op=ALU.add), reads=[bbis], writes=[bbis])
                                cn = cnt
                            else:
                                cn = cnts[:, 0:1]
                            dve.op(lambda e, it=it, cn=cn: e.tensor_scalar(out=dd, in0=cn, scalar1=TOPK, scalar2=hwt[:, it:it + 1], op0=ALU.is_ge, op1=ALU.mult),
                                   reads=[bbis], writes=[bbis])
                            dve.op(lambda e: e.tensor_tensor(out=lo, in0=lo, in1=dd, op=ALU.add), reads=[bbis], writes=[bbis])
                            dve.op(lambda e, it=it: e.tensor_tensor(out=mid, in0=lo, in1=hwt[:, it + 1:it + 2], op=ALU.add), reads=[bbis], writes=[bbis])
                        for a in range(0, NK, 4096):
                            wdt = min(4096, NK - a)
                            dve.op(lambda e, a=a, wdt=wdt: e.tensor_scalar(out=Sc[:, a:a + wdt], in0=Sc[:, a:a + wdt], scalar1=lo, scalar2=None, op0=ALU.subtract),
                                    reads=[bS, bbis], writes=[bS])
                    def pA(bl):
                        l = gi * 4 + bl
                        NK = (8 * l + 8) * 128
                        nkb = 8 * l + 8
                        tsl = slice(bl * 128, (bl + 1) * 128)
                        K.mute = ("A" not in STAGES)
                        dve.op(lambda e: e.memset(ps[3][:, :], 0.0), writes=[pb[3]])
                        for c0 in range(0, NK, 512):
                            K.mute = ("A" not in STAGES)
                            ki = load_kv("A", c0)
                            for kb4 in range(4):
                                kb = c0 // 128 + kb4
                                j = kb % 2
                                ksl = slice(kb4 * 128, (kb4 + 1) * 128)
                                K.mute = ("A" not in STAGES) or ASUB < 2
                                pe.op(lambda e, kb=kb: e.matmul(ps[2][:, 0:128], lhsT=Sc[:, kb * 128:(kb + 1) * 128], rhs=identf, start=True, stop=True), reads=[bS, b_cst], writes=[pb[2]])
                                K.mute = ("A" not in STAGES) or ASUB < 3
                                for h in range(4):
                                    pr, off = h // 2, 64 * (h % 2)
                                    pe.op(lambda e, h=h, pr=pr, off=off, ksl=ksl, j=j: e.matmul(ps[j][:, h * 128:(h + 1) * 128], lhsT=kt[ki][:, pr, ksl],
                                                                                                rhs=qAz[:, h, tsl], start=True, stop=True),
                                          reads=[kvb[ki], bq], writes=[pb[j]])
                                K.mute = ("A" not in STAGES) or ASUB < 4
                                act.op(lambda e, j=j: e.activation(out=Wt[j][:], in_=ps[j][:, :], func=AF.Exp, scale=0.125), reads=[pb[j]], writes=[bW[j]])
                                K.mute = ("A" not in STAGES) or ASUB < 5
                                dve.op(lambda e, j=j: e.scalar_tensor_tensor(out=Lt[j][:].rearrange("p (h t) -> p h t", h=4),
                                                                             in0=ps[2][:, 0:128].unsqueeze(1).to_broadcast([128, 4, 128]), scalar=0.0,
                                                                             in1=Wt[j][:].rearrange("p (h t) -> p h t", h=4), op0=ALU.is_ge, op1=ALU.mult),
                                       reads=[pb[2], bW[j]], writes=[bL[j]])
                                K.mute = ("A" not in STAGES) or ASUB < 6
                                for h in range(4):
                                    pr, off = h // 2, 64 * (h % 2)
                                    pe.op(lambda e, h=h, pr=pr, off=off, kb4=kb4, kb=kb, j=j: e.matmul(ps[3][off:off + 64, pr * 128:(pr + 1) * 128], lhsT=vt[ki][:, kb4, h * 64:(h + 1) * 64],
                                                                                                      rhs=Lt[j][:, h * 128:(h + 1) * 128], start=False, stop=(kb == nkb - 1)),
                                          reads=[kvb[ki], bL[j]], writes=[pb[3]])
                                    pe.op(lambda e, h=h, pr=pr, off=off, kb=kb, j=j: e.matmul(ps[3][off:off + 64, 256 + pr * 128:256 + (pr + 1) * 128], lhsT=onesb[:, 0:64],
                                                                                             rhs=Lt[j][:, h * 128:(h + 1) * 128], start=False, stop=(kb == nkb - 1)),
                                          reads=[b_cst, bL[j]], writes=[pb[3]])
                        K.mute = ("A" not in STAGES) or ASUB < 7
                        dve.op(lambda e: e.reciprocal(out=fin[:, 0:256], in_=ps[3][:, 256:512]), reads=[pb[3]], writes=[bfin])
                        dve.op(lambda e: e.tensor_tensor(out=fin[:, 256:512], in0=ps[3][:, 0:256], in1=fin[:, 0:256], op=ALU.mult), reads=[pb[3], bfin], writes=[bfin])
                        pool.op(lambda e: e.tensor_tensor(out=oT[:, 0, :, tsl], in0=fin[:, 256:512].rearrange("p (a t) -> p a t", a=2), in1=gsl[:, 0:2, tsl], op=ALU.mult),
                                reads=[bfin, bgs], writes=[boT])

                    def pC(bl):
                        l = gi * 4 + bl
                        NK = (8 * l + 8) * 128
                        nkb = 8 * l + 8
                        tsl = slice(bl * 128, (bl + 1) * 128)
                        K.mute = ("C" not in STAGES)
                        first = True
                        for c0 in range(NK - 512, -1, -512):
                            K.mute = ("C" not in STAGES)
                            ki = load_kv("C", c0)
                            for kb4 in range(3, -1, -1):
                                kb = c0 // 128 + kb4
                                j = kb % 2
                                o = kb - 8 * l
                                ksl = slice(kb4 * 128, (kb4 + 1) * 128)
                                K.mute = ("C" not in STAGES) or CSUB < 2
                                for h in range(4):
                                    pr, off = h // 2, 64 * (h % 2)
                                    pe.op(lambda e, h=h, pr=pr, off=off, ksl=ksl, j=j: e.matmul(ps[j][:, h * 128:(h + 1) * 128], lhsT=kt[ki][:, pr, ksl],
                                                                                                rhs=qCz[:, h, tsl], start=True, stop=True),
                                          reads=[kvb[ki], bq], writes=[pb[j]])
                                K.mute = ("C" not in STAGES) or CSUB < 3
                                act.op(lambda e, j=j: e.activation(out=Et[j][:], in_=ps[j][:, :], func=AF.Exp, scale=0.125), reads=[pb[j]], writes=[bE[j]])
                                K.mute = ("C" not in STAGES) or CSUB < 4
                                act.op(lambda e, j=j: e.activation(out=Lt[j][:], in_=Et[j][:], func=AF.Ln, bias=cc("one"), scale=1.0), reads=[bE[j], b_cst], writes=[bL[j]])
                                K.mute = ("C" not in STAGES) or CSUB < 5
                                if o >= 0:
                                    for hh_ in range(4):
                                        hs = slice(hh_ * 128, (hh_ + 1) * 128)
                                        pool.op(lambda e, j=j, hs=hs, o=o: e.tensor_tensor(out=Et[j][:, hs], in0=Et[j][:, hs], in1=mst[:, o * 128:(o + 1) * 128], op=ALU.mult),
                                                reads=[bE[j], b_vec], writes=[bE[j]])
                                        pool.op(lambda e, j=j, hs=hs, o=o: e.tensor_tensor(out=Lt[j][:, hs], in0=Lt[j][:, hs], in1=mst[:, o * 128:(o + 1) * 128], op=ALU.mult),
                                                reads=[bL[j], b_vec], writes=[bL[j]])
                                K.mute = ("C" not in STAGES) or CSUB < 6
                                pe.op(lambda e, j=j, first=first: e.matmul(ps[4 + j][:, :], lhsT=trib, rhs=Lt[j][:], start=True, stop=first), reads=[b_cst, bL[j]], writes=[pb[4 + j]])
                                K.mute = ("C" not in STAGES) or CSUB < 7
                                if not first:
                                    pe.op(lambda e, j=j: e.matmul(ps[4 + j][:, :], lhsT=onesb[0:1, :], rhs=car[0:1, 0:512], start=False, stop=True), reads=[b_cst, bcar], writes=[pb[4 + j]])
                                K.mute = ("C" not in STAGES) or CSUB < 8
                                if kb > 0:
                                    act.op(lambda e, j=j: e.copy(out=car[0:32, 0:512], in_=ps[4 + j][0:32, :]), reads=[pb[4 + j]], writes=[bcar])
                                K.mute = ("C" not in STAGES) or CSUB < 9
                                act.op(lambda e, j=j: e.activation(out=Xt[j][:], in_=ps[4 + j][:, :], func=AF.Exp, scale=-1.0), reads=[pb[4 + j]], writes=[bX[j]])
                                K.mute = ("C" not in STAGES) or CSUB < 10
                                pool.op(lambda e, j=j: e.tensor_tensor(out=Wt[j][:], in0=Et[j][:], in1=Xt[j][:], op=ALU.mult), reads=[bE[j], bX[j]], writes=[bW[j]])
                                K.mute = ("C" not in STAGES) or CSUB < 11
                                for h in range(4):
                                    pr, off = h // 2, 64 * (h % 2)
                                    pe.op(lambda e, h=h, pr=pr, off=off, kb4=kb4, kb=kb, j=j, first=first: e.matmul(ps[3][off:off + 64, pr * 128:(pr + 1) * 128],
                                                                                                                     lhsT=vt[ki][:, kb4, h * 64:(h + 1) * 64], rhs=Wt[j][:, h * 128:(h + 1) * 128],
                                                                                                                     start=False, stop=(kb == 0)),
                                          reads=[kvb[ki], bW[j]], writes=[pb[3]])
                                first = False
                        K.mute = ("C" not in STAGES) or CSUB < 12
                        dve.op(lambda e: e.tensor_tensor(out=oT[:, 2, :, tsl], in0=ps[3][:, 0:256].rearrange("p (a t) -> p a t", a=2), in1=gsl[:, 4:6, tsl], op=ALU.mult),
                               reads=[pb[3], bgs], writes=[boT])
                    p1(0)
                    for bl in range(4):
                        pA(bl)
                        K.mute = ("C" not in STAGES)
                        dve.op(lambda e: e.memset(ps[3][:, :], 0.0), writes=[pb[3]])
                        if bl < 3:
                            p1(bl + 1)
                        pC(bl)
                    K.barrier()

                K.mute = ("post" not in STAGES)
                with ExitStack() as spo:
                    xg = [T(f"xg{i}", [128, 512], F32, spo) for i in range(2)]
                    bxg = [Buf(), Buf()]
                    dxg = [DSem(K, "dxg0"), DSem(K, "dxg1")]
                    mg = T("mg", [128, 8, 512], F32, spo)
                    mgb = T("mgb", [128, 8, 512], BF16, spo)
                    yt = T("yt", [128, 8, 512], F32, spo)
                    ysq = [T(f"ysq{i}", [128, 512], BF16, spo) for i in range(2)]
                    gt = [T(f"gt{i}", [128, 512], F32, spo) for i in range(2)]
                    tt = [T(f"tt{i}", [128, 512], F32, spo) for i in range(2)]
                    r2 = T("r2", [128, 512], F32, spo)
                    bmg_, bmgb, byt, br2 = Buf(), Buf(), Buf(), Buf()
                    bysq = [Buf(), Buf()]
                    bgt = [Buf(), Buf()]
                    btt = [Buf(), Buf()]
                    wbr = T("wbr", [128, 8, 1024], BF16, spo)
                    wbrb = Buf()
                    dwbr = DSem(K, "dwbr")
                    for k in range(8):
                        pool.dma(wbr[:, k, :], w_branch[k // 2, (k % 2) * 128:(k % 2 + 1) * 128, :], dwbr, writes=[wbrb] if k == 0 else [])
                    wbrb.w = (dwbr.h, dwbr.cnt, None)
                    q = 0
                    for n in range(4):
                        wm, wmb = load_w(wext(C_WM + n * 1024, 1024), 8, 1024)
                        for dt_ in range(8):
                            j = q % 2
                            q += 1
                            for kk in range(2):
                                pe.op(lambda e, kk=kk, j=j, n=n, dt_=dt_: e.matmul(ps[j][:, :], lhsT=wbr[:, n * 2 + kk, dt_ * 128:(dt_ + 1) * 128], rhs=oT[:, n, kk, :],
                                                                                  start=(kk == 0), stop=(kk == 1)), reads=[wbrb, boT], writes=[pb[j]])
                            proj_fm(wm, wmb, dt_ * 128, 128, hT, bh, 512, 2 + j)
                            act.op(lambda e, j=j, n=n, dt_=dt_: e.activation(out=gt[j][:], in_=ps[2 + j][:, :], func=AF.Sigmoid, bias=bmg[:, n * 8 + dt_:n * 8 + dt_ + 1], scale=1.0),
                                   reads=[pb[2 + j], b_vec], writes=[bgt[j]])
                            if n == 0:
                                dve.op(lambda e, j=j, dt_=dt_: e.tensor_tensor(out=mg[:, dt_, :], in0=ps[j][:, :], in1=gt[j][:], op=ALU.mult), reads=[pb[j], bgt[j]], writes=[bmg_])
                            else:
                                dve.op(lambda e, j=j: e.tensor_tensor(out=tt[j][:], in0=ps[j][:, :], in1=gt[j][:], op=ALU.mult), reads=[pb[j], bgt[j]], writes=[btt[j]])
                                pool.op(lambda e, j=j, dt_=dt_: e.tensor_tensor(out=mg[:, dt_, :], in0=mg[:, dt_, :], in1=tt[j][:], op=ALU.add), reads=[btt[j], bmg_], writes=[bmg_])
                    pool.op(lambda e: e.tensor_copy(out=mgb[:], in_=mg[:]), reads=[bmg_], writes=[bmgb])
                    wo, wob = load_w(lambda k: w_out[k * 128:(k + 1) * 128, :], 8, 1024)
                    for dt_ in range(8):
                        j = dt_ % 2
                        proj_fm(wo, wob, dt_ * 128, 128, mgb, bmgb, 512, j)
                        act.op(lambda e, j=j, dt_=dt_: e.copy(out=yt[:, dt_, :], in_=ps[j][:, :]), reads=[pb[j]], writes=[byt])
                        act.op(lambda e, j=j, dt_=dt_: e.activation(out=ysq[j][:], in_=ps[j][:, :], func=AF.Square), reads=[pb[j]], writes=[bysq[j]])
                        pe.op(lambda e, j=j, dt_=dt_: e.matmul(ps[4][:, :], lhsT=onesb, rhs=ysq[j][:], start=(dt_ == 0), stop=(dt_ == 7)), reads=[bysq[j], b_cst], writes=[pb[4]])
                    act.op(lambda e: e.activation(out=r2[:], in_=ps[4][:, :], func=AF.Ln, bias=cc("epsk"), scale=1.0), reads=[pb[4], b_cst], writes=[br2])
                    act.op(lambda e: e.activation(out=r2[:], in_=r2[:], func=AF.Exp, scale=-0.5), reads=[br2], writes=[br2])
                    for k in range(8):
                        dve.op(lambda e, k=k: e.scalar_tensor_tensor(out=yt[:, k, :], in0=yt[:, k, :], scalar=g32[:, 8 + k:9 + k], in1=r2[:], op0=ALU.mult, op1=ALU.mult),
                               reads=[byt, br2, b_vec], writes=[byt])
                        sp.dma(xg[k % 2][:], xT_own[k * 128:(k + 1) * 128, tg0:tg0 + 512], dxg[k % 2], writes=[bxg[k % 2]])
                        pool.op(lambda e, k=k: e.tensor_tensor(out=yt[:, k, :], in0=yt[:, k, :], in1=xg[k % 2][:], op=ALU.add), reads=[byt, bxg[k % 2]], writes=[byt])
                        pool.op(lambda e, k=k: e.tensor_copy(out=mgb[:, k, :], in_=yt[:, k, :]), reads=[byt], writes=[bmgb])
                    wp, wpb = load_w(lambda k: w_pg[k * 128:(k + 1) * 128, :], 8, 1024)
                    wl, wlb = load_w(lambda k: w_ple[k * 128:(k + 1) * 128, :], 2, 1024)
                    for dt_ in range(8):
                        j = dt_ % 2
                        proj_fm(wp, wpb, dt_ * 128, 128, mgb, bmgb, 512, j)
                        for kk in range(2):
                            pe.op(lambda e, kk=kk, j=j, dt_=dt_: e.matmul(ps[2 + j][:, :], lhsT=wl[:, kk, dt_ * 128:(dt_ + 1) * 128], rhs=pTb[:, kk, :], start=(kk == 0), stop=(kk == 1)),
                                  reads=[wlb, bpT], writes=[pb[2 + j]])
                        act.op(lambda e, j=j: e.activation(out=gt[j][:], in_=ps[j][:, :], func=AF.Sigmoid), reads=[pb[j]], writes=[bgt[j]])
                        dve.op(lambda e, j=j: e.tensor_tensor(out=tt[j][:], in0=ps[2 + j][:, :], in1=gt[j][:], op=ALU.mult), reads=[pb[2 + j], bgt[j]], writes=[btt[j]])
                        pool.op(lambda e, j=j, dt_=dt_: e.tensor_tensor(out=mg[:, dt_, :], in0=yt[:, dt_, :], in1=tt[j][:], op=ALU.add), reads=[btt[j], byt], writes=[bmg_])
                    for k in range(8):
                        sp.dma(out[k * 128:(k + 1) * 128, tg0:tg0 + 512], mg[:, k, :], osem2, reads=[bmg_] if k == 0 else [])
                    tk = (osem2.h, osem2.cnt, None)
                    bmg_.r[osem2] = tk
                    K.barrier()
        K.mute = False
        K.barrier()
    return nc


_NC_CACHE = {}


def _layer_inputs(layer, core, xfull_T, x_rows, inp, consts_np):
    blocks = [8 * l + core for l in range(LB)]
    own_idx = np.concatenate([np.arange(b * 128, (b + 1) * 128) for b in blocks])
    x_own = x_rows[own_idx]
    halo = np.zeros((32, D), np.float32)
    for l, b in enumerate(blocks):
        if b > 0:
            halo[2 * l:2 * l + 2] = x_rows[b * 128 - 2:b * 128]
    pos = np.asarray(inp["positions"])[0].astype(np.int32)
    cm, ms = make_masks(core)
    vecs = np.zeros((128, 64), np.float32)
    vecs[:, 0:8] = np.asarray(inp["g_pre"])[layer].reshape(8, 128).T
    vecs[:, 8:16] = np.asarray(inp["g_post"])[layer].reshape(8, 128).T
    vecs[:, 16:48] = np.asarray(inp["b_merge"])[layer].reshape(32, 128).T
    cwl = np.asarray(inp["conv_w"])[layer]
    vecs[:, 48:54] = cwl.reshape(3, 2, 128).transpose(2, 1, 0).reshape(128, 6)
    vecs[:, 54:56] = np.asarray(inp["conv_b"])[layer].reshape(2, 128).T
    lnb = np.concatenate([np.broadcast_to(np.asarray(inp["ln_g"])[layer][None, :], (128, 256)),
                          np.broadcast_to(np.asarray(inp["ln_b"])[layer][None, :], (128, 256))], axis=1)
    wsT = np.asarray(inp["w_spatial"])[layer].transpose(2, 0, 1).reshape(128, 512)
    return {
        "xT_full": xfull_T,
        "xT_own": np.ascontiguousarray(x_own.T),
        "xT_halo": np.ascontiguousarray(halo.T),
        "pT_own": np.ascontiguousarray(np.asarray(inp["p"])[layer, 0][own_idx].T),
        "pos_full": np.ascontiguousarray(pos[None, :]),
        "pos_own": np.ascontiguousarray(pos[own_idx][None, :]),
        "w_ext": inp["_w_ext"][layer],
        "w_branch": np.ascontiguousarray(np.asarray(inp["w_branch"])[layer]),
        "w_out": np.ascontiguousarray(np.asarray(inp["w_out"])[layer]),
        "w_ple": np.ascontiguousarray(np.asarray(inp["w_ple"])[layer]),
        "w_pg": np.ascontiguousarray(np.asarray(inp["w_ple_gate"])[layer]),
        "wsT": np.ascontiguousarray(wsT),
        "vecs": vecs,
        "lnb": np.ascontiguousarray(lnb.astype(np.float32)),
        "bsp": np.ascontiguousarray(np.asarray(inp["b_spatial"])[layer]),
        "consts": consts_np,
        "cm": cm, "ms": ms,
    }


def kernel(**inputs):
    inp = dict(inputs)
    w_in = np.asarray(inp["w_in"])
    inp["_w_ext"] = [np.ascontiguousarray(w_in[l][:, ALL_COLS]) for l in range(w_in.shape[0])]
    consts_np = make_consts()
    if "nc" not in _NC_CACHE:
        _NC_CACHE["nc"] = build_program()
    nc = _NC_CACHE["nc"]
    x_rows = np.asarray(inp["x"])[0].astype(np.float32)
    depth = w_in.shape[0]
    for layer in range(depth):
        xfull_T = np.ascontiguousarray(x_rows.T)
        in_maps = [_layer_inputs(layer, c, xfull_T, x_rows, inp, consts_np) for c in range(NCORES)]
        res = run_bass_kernel_spmd(nc, in_maps, core_ids=list(range(NCORES)))
        x_new = np.empty_like(x_rows)
        for c in range(NCORES):
            o = np.asarray(res.results[c]["xT_next"]).T
            for l in range(LB):
                b = 8 * l + c
                x_new[b * 128:(b + 1) * 128] = o[l * 128:(l + 1) * 128]
        x_rows = x_new
    return x_rows[None].astype(np.float32)
```

```python
import math
from contextlib import ExitStack

import numpy as np
import concourse.bass as bass
import concourse.mybir as mybir
from concourse.bass_utils import run_bass_kernel_spmd

F32 = mybir.dt.float32
BF16 = mybir.dt.bfloat16
I32 = mybir.dt.int32
AF = mybir.ActivationFunctionType
ALU = mybir.AluOpType
AX = mybir.AxisListType

NCORES = 8
S = 16384
D = 1024
NBLK = S // 128
LB = NBLK // NCORES
OWN = LB * 128
NG = 4
EPS = 1e-6
NIT = 22
TOPK = 256.0
IDX_W_SCALE = 128 ** -0.5
PI = math.pi

O_AQ, O_AK, O_AV, O_IQ, O_IK, O_IW = 0, 256, 512, 768, 896, 928
O_BB, O_BC, O_BX, O_CQ, O_CK, O_CV, O_DU, O_DV, O_GP, O_MG = 932, 1188, 1444, 1700, 1956, 2212, 2468, 2724, 2980, 4004


def _swap(off, n, hd):
    idx = []
    for h in range(n // hd):
        b = off + h * hd
        idx += list(range(b + hd // 2, b + hd)) + list(range(b, b + hd // 2))
    return idx


def _r(off, n):
    return list(range(off, off + n))


W1_COLS = _r(O_AK, 256) + _swap(O_AK, 256, 64) + _r(O_CK, 256) + _r(O_IK, 32) + _swap(O_IK, 32, 32) + _r(O_AV, 256) + _r(O_CV, 256)
WA_COLS = _r(O_AQ, 256) + _swap(O_AQ, 256, 64) + _r(O_CQ, 256) + _r(O_IQ, 128) + _swap(O_IQ, 128, 32)
WB_COLS = _r(O_BB, 256) + _r(O_BC, 256) + _r(O_BX, 256) + _r(O_DU, 256)
WC_COLS = _r(O_DV, 256) + _r(O_IW, 4)
WD_COLS = _r(O_GP, 1024)
WM_COLS = _r(O_MG, 4096)
C_W1 = 0
C_WA = C_W1 + len(W1_COLS)
C_WB = C_WA + len(WA_COLS)
C_WC = C_WB + len(WB_COLS)
C_WD = C_WC + len(WC_COLS)
C_WM = C_WD + len(WD_COLS)
NWCOL = C_WM + len(WM_COLS)
ALL_COLS = np.array(W1_COLS + WA_COLS + WB_COLS + WC_COLS + WD_COLS + WM_COLS, dtype=np.int64)

CO = {}
_off = 0
for _n, _w in [("ident", 128), ("tri", 128), ("ones", 128), ("trilst", 128), ("invfA", 1), ("sgnA", 1), ("nbA", 1),
               ("invfI", 1), ("sgnI", 1), ("nbI", 1), ("negpi", 1), ("one", 1), ("hw", NIT + 1), ("eg", 256), ("epsk", 1), ("eps", 1), ("pospi", 1)]:
    CO[_n] = (_off, _w)
    _off += _w
NCONST = _off


def make_consts():
    c = np.zeros((128, NCONST), np.float32)
    p = np.arange(128)
    c[:, CO["ident"][0]:CO["ident"][0] + 128] = np.eye(128)
    c[:, CO["tri"][0]:CO["tri"][0] + 128] = (p[:, None] >= p[None, :])
    c[:, CO["ones"][0]:CO["ones"][0] + 128] = 1.0
    c[:, CO["trilst"][0]:CO["trilst"][0] + 128] = (p[:, None] <= p[None, :])
    jA = p % 32
    c[:, CO["invfA"][0]] = (10000.0 ** (-(2.0 * jA) / 64.0)).astype(np.float32)
    sg = np.where((p % 64) < 32, -1.0, 1.0)
    c[:, CO["sgnA"][0]] = -sg
    c[:, CO["nbA"][0]] = PI * sg
    jI = p % 16
    c[:, CO["invfI"][0]] = (10000.0 ** (-(2.0 * jI) / 32.0)).astype(np.float32)
    sgi = np.where((p % 32) < 16, -1.0, 1.0)
    c[:, CO["sgnI"][0]] = -sgi
    c[:, CO["nbI"][0]] = PI * sgi
    c[:, CO["negpi"][0]] = -PI
    c[:, CO["pospi"][0]] = PI
    c[:, CO["one"][0]] = 1.0
    c[:, CO["epsk"][0]] = 1024.0 * EPS
    c[:, CO["eps"][0]] = EPS
    c[:, CO["hw"][0]:CO["hw"][0] + NIT + 1] = 2.0 ** (-(np.arange(NIT + 1) + 1.0))
    eg = np.zeros((128, 2, 128), np.float32)
    for g in range(4):
        eg[g, g // 2, (g % 2) * 64:(g % 2) * 64 + 64] = 1.0
    c[:, CO["eg"][0]:CO["eg"][0] + 256] = eg.reshape(128, 256)
    return c


def make_masks(core):
    t = np.arange(128)[:, None]
    s = np.arange(128)[None, :]
    cm = np.zeros((128, 8, 128), np.float32)
    ms = np.zeros((128, 8, 4, 128), np.float32)
    for o in range(8):
        if o < core:
            valid_le = np.ones((128, 128), bool)
            valid_lt = np.ones((128, 128), bool)
        elif o == core:
            valid_le = s <= t
            valid_lt = s < t
        else:
            valid_le = np.zeros((128, 128), bool)
            valid_lt = np.zeros((128, 128), bool)
        cm[:, o, :] = np.where(valid_le, 0.0, -1e30)
        ms[:, o, :, :] = valid_lt.T[:, None, :]
    return cm.reshape(128, 1024), np.ascontiguousarray(ms[:, :, 0, :]).reshape(128, 1024)


class Buf:
    __slots__ = ("w", "r")

    def __init__(self):
        self.w = None
        self.r = {}


class DSem:
    def __init__(self, K, name):
        self.K = K
        self.name = name
        self.h = K.newsem(name)
        self.cnt = 0

    def bump(self):
        if self.cnt >= 32000:
            self.h = self.K.newsem(self.name)
            self.cnt = 0
        self.cnt += 16
        return (self.h, self.cnt, None)


class Eng:
    def __init__(self, K, eng, name, sew=True):
        self.K, self.e, self.name, self.sew = K, eng, name, sew
        self.sem = K.newsem(name)
        self.cnt = 0
        self.waited = {}
        self.last = None

    def wait(self, tok):
        if tok is None:
            return
        sem, val, owner = tok
        if owner is self and not self.sew:
            return
        key = id(sem)
        if self.waited.get(key, 0) >= val:
            return
        self.e.wait_ge(sem, val)
        self.waited[key] = val

    def _deps(self, reads, writes):
        for b in reads:
            self.wait(b.w)
        for b in writes:
            self.wait(b.w)
            for own, t in b.r.items():
                if own is not self:
                    self.wait(t)

    def _mark(self, tok, reads, writes, owner):
        for b in reads:
            b.r[owner] = tok
        for b in writes:
            b.w = tok
            b.r = {}

    def op(self, fn, reads=(), writes=()):
        if self.K.mute:
            return None
        self._deps(reads, writes)
        if self.cnt >= 30000:
            self.sem = self.K.newsem(self.name)
            self.cnt = 0
        ins = fn(self.e)
        self.cnt += 1
        ins.then_inc(self.sem, 1)
        tok = (self.sem, self.cnt, self)
        self._mark(tok, reads, writes, self)
        self.last = tok
        return tok

    def dma(self, out, in_, dsem, reads=(), writes=()):
        if self.K.mute:
            return None
        self._deps(reads, writes)
        ins = self.e.dma_start(out=out, in_=in_)
        tok = dsem.bump()
        ins.then_inc(tok[0], 16)
        self._mark(tok, reads, writes, dsem)
        self.K.dsems[id(dsem)] = dsem
        return tok


class Kern:
    def __init__(self, nc, stack):
        self.nc, self.stack = nc, stack
        self._uid = 0
        self.mute = False
        self.dsems = {}
        self.pe = Eng(self, nc.tensor, "pe", sew=False)
        self.act = Eng(self, nc.scalar, "act")
        self.dve = Eng(self, nc.vector, "dve")
        self.pool = Eng(self, nc.gpsimd, "pool")
        self.sp = Eng(self, nc.sync, "sp")
        self.engs = [self.pe, self.act, self.dve, self.pool, self.sp]

    def newsem(self, name):
        self._uid += 1
        return self.stack.enter_context(self.nc.semaphore(f"{name}_{self._uid}"))

    def barrier(self):
        toks = [e.last for e in self.engs if e.last is not None]
        for d in self.dsems.values():
            if d.cnt:
                toks.append((d.h, d.cnt, None))
        for e in self.engs:
            for t in toks:
                if t[2] is not e:
                    e.wait(t)


import os
P1_CHUNKS = int(os.environ.get('P1_CHUNKS', S // 512))
NGROUPS = int(os.environ.get('NGROUPS', NG))
ASUB = int(os.environ.get('ASUB', 9))
CSUB = int(os.environ.get('CSUB', 99))
STAGES = os.environ.get('STAGES', 'gate,bd,d,q,score,bis,A,C,post').split(',')


def build_program():
    nc = bass.Bass("TRN2", target_bir_lowering=False)

    def din(name, shape, dt=F32):
        return nc.dram_tensor(name, list(shape), dt, kind="ExternalInput").ap()

    xT_full = din("xT_full", [D, S])
    xT_own = din("xT_own", [D, OWN])
    xT_halo = din("xT_halo", [D, 32])
    pT_own = din("pT_own", [256, OWN])
    pos_full = din("pos_full", [1, S], I32)
    pos_own = din("pos_own", [1, OWN], I32)
    w_ext = din("w_ext", [D, NWCOL])
    w_branch = din("w_branch", [4, 256, D])
    w_out = din("w_out", [D, D])
    w_ple = din("w_ple", [256, D])
    w_pg = din("w_pg", [D, D])
    wsT = din("wsT", [128, 4 * 128])
    vecs = din("vecs", [128, 64])
    lnb = din("lnb", [128, 512])
    bsp = din("bsp", [4, 128])
    consts = din("consts", [128, NCONST])
    cm_in = din("cm", [128, 1024])
    ms_in = din("ms", [128, 1024])
    out = nc.dram_tensor("xT_next", [D, OWN], F32, kind="ExternalOutput").ap()

    KTA = nc.dram_tensor("KTA", [2, 128, S], BF16)
    KTC = nc.dram_tensor("KTC", [2, 128, S], BF16)
    IKT = nc.dram_tensor("IKT", [32, S], BF16)
    VAC = nc.dram_tensor("VAC", [S, 512], BF16)

    with ExitStack() as stack:
        K = Kern(nc, stack)
        pe, act, dve, pool, sp = K.pe, K.act, K.dve, K.pool, K.sp

        def T(name, shape, dt=F32, st=stack):
            K._uid += 1
            return st.enter_context(nc.sbuf_tensor(f"{name}_{K._uid}", list(shape), dt))

        cst = T("cst", [128, NCONST])
        cstb = T("cstb", [128, 3 * 128 + 256], BF16)
        vec = T("vec", [128, 64])
        lnbt = T("lnbt", [128, 512])
        bspb = T("bspb", [4, 128], BF16)
        wst = T("wst", [128, 512], BF16)
        wsf = T("wsf", [128, 512])
        cmt = T("cmt", [128, 1024])
        mst = T("mst", [128, 1024])
        g32 = T("g32", [128, 16])
        yhalo = T("yhalo", [128, 2, 32])
        ps = [stack.enter_context(nc.psum_tensor(f"ps{i}", [128, 512], F32)) for i in range(8)]
        pb = [Buf() for _ in range(8)]
        b_cst, b_vec = Buf(), Buf()
        ld = DSem(K, "ld")

        def cc(name, a=0, n=None):
            o, w = CO[name]
            n = w if n is None else n
            return cst[:, o + a:o + a + n]

        identb = cstb[:, 0:128]
        trib = cstb[:, 128:256]
        onesb = cstb[:, 256:384]
        egb = cstb[:, 384:640]

        sp.dma(cst[:], consts, ld, writes=[b_cst])
        sp.dma(vec[:], vecs, ld, writes=[b_vec])
        sp.dma(lnbt[:], lnb, ld, writes=[b_vec])
        sp.dma(wsf[:], wsT, ld, writes=[b_vec])
        sp.dma(cmt[:], cm_in, ld, writes=[b_vec])
        sp.dma(mst[:], ms_in, ld, writes=[b_vec])
        pool.dma(bspb[:], bsp, ld, writes=[b_vec])
        dve.op(lambda e: e.tensor_copy(out=cstb[:, 0:384], in_=cst[:, 0:384]), reads=[b_cst], writes=[b_cst])
        dve.op(lambda e: e.tensor_copy(out=cstb[:, 384:640], in_=cc("eg")), reads=[b_cst], writes=[b_cst])
        for g in range(4):
            dve.op(lambda e, g=g: e.tensor_tensor(out=wst[:, g * 128:(g + 1) * 128], in0=wsf[:, g * 128:(g + 1) * 128],
                                                  in1=cc("trilst"), op=ALU.mult), reads=[b_vec, b_cst], writes=[b_vec])
        dve.op(lambda e: e.tensor_scalar(out=g32[:], in0=vec[:, 0:16], scalar1=32.0, scalar2=None, op0=ALU.mult),
               reads=[b_vec], writes=[b_vec])
        K.barrier()
        bmg = vec[:, 16:48]
        cw = vec[:, 48:54]
        cb = vec[:, 54:56]
        iota_dummy = None

        wbuf = [T(f"wbuf{i}", [128, 8, 1024], BF16) for i in range(2)]
        wb_b = [Buf(), Buf()]
        wsem = [DSem(K, "w0"), DSem(K, "w1")]
        wctr = [0]

        def load_w(src_ap_fn, nk, ncols):
            i = wctr[0] % 2
            wctr[0] += 1
            for k in range(nk):
                pool.dma(wbuf[i][:, k, 0:ncols], src_ap_fn(k), wsem[i], writes=[wb_b[i]] if k == 0 else [])
                if k > 0:
                    wb_b[i].w = (wsem[i].h, wsem[i].cnt, None)
            wb_b[i].w = (wsem[i].h, wsem[i].cnt, None)
            return wbuf[i], wb_b[i]

        def wext(c0, ncols):
            return lambda k: w_ext[k * 128:(k + 1) * 128, c0:c0 + ncols]

        def make_h(st, name, x_src_fn, ntok, xkeep=None, tst=None):
            tst = st if tst is None else tst
            hT = T(name + "_h", [128, 8, ntok], BF16, st)
            xt = T(name + "_x", [128, 8, ntok], F32, tst)
            sq = T(name + "_sq", [128, 8, ntok], BF16, tst)
            rr = T(name + "_r", [128, ntok], F32, tst)
            bx_, bsq, bh, br = Buf(), Buf(), Buf(), Buf()
            xs = DSem(K, name + "_xs")
            return dict(xt=xt, sq=sq, hT=hT, rr=rr, bx=bx_, bsq=bsq, bh=bh, br=br, xs=xs, ntok=ntok)

        def run_h(hh, x_src_fn, pbank):
            ntok = hh["ntok"]
            xt, sq, hT, rr = hh["xt"], hh["sq"], hh["hT"], hh["rr"]
            for k in range(8):
                sp.dma(xt[:, k, :], x_src_fn(k), hh["xs"], writes=[hh["bx"]] if k == 0 else [])
            hh["bx"].w = (hh["xs"].h, hh["xs"].cnt, None)
            act.op(lambda e: e.activation(out=sq[:], in_=xt[:], func=AF.Square), reads=[hh["bx"]], writes=[hh["bsq"]])
            for k in range(8):
                pe.op(lambda e, k=k: e.matmul(ps[pbank][:, 0:ntok], lhsT=onesb, rhs=sq[:, k, :], start=(k == 0), stop=(k == 7)),
                      reads=[hh["bsq"], b_cst], writes=[pb[pbank]])
            act.op(lambda e: e.activation(out=rr[:], in_=ps[pbank][:, 0:ntok], func=AF.Ln, bias=cc("epsk"), scale=1.0), reads=[pb[pbank], b_cst], writes=[hh["br"]])
            act.op(lambda e: e.activation(out=rr[:], in_=rr[:], func=AF.Exp, scale=-0.5), reads=[hh["br"]], writes=[hh["br"]])
            for k in range(8):
                dve.op(lambda e, k=k: e.scalar_tensor_tensor(out=hT[:, k, :], in0=xt[:, k, :], scalar=g32[:, k:k + 1], in1=rr[:],
                                                              op0=ALU.mult, op1=ALU.mult),
                        reads=[hh["bx"], hh["br"], b_vec], writes=[hh["bh"]])

        def rope_tables(st, name, pos_src, ntok, npart, invf, sgn, nb):
            pi_ = T(name + "_pi", [npart, ntok], I32, st)
            pf = T(name + "_pf", [npart, ntok], F32, st)
            a1 = T(name + "_a1", [npart, ntok], F32, st)
            cos = T(name + "_cos", [npart, ntok], F32, st)
            sin = T(name + "_sin", [npart, ntok], F32, st)
            return dict(pi=pi_, pf=pf, a1=a1, cos=cos, sin=sin, b=Buf(), bt=Buf(), ds=DSem(K, name + "_d"),
                        npart=npart, invf=invf, sgn=sgn, nb=nb, ntok=ntok)

        def run_rope_tables(rt, pos_src):
            n = rt["npart"]
            sp.dma(rt["pi"][:], pos_src.partition_broadcast(n), rt["ds"], writes=[rt["b"]])
            dve.op(lambda e: e.tensor_copy(out=rt["pf"][:], in_=rt["pi"][:]), reads=[rt["b"]], writes=[rt["b"]])
            C1 = 6.28125
            C2 = 2 * PI - C1
            pf, a1, pi_, cos, sin = rt["pf"], rt["a1"], rt["pi"], rt["cos"], rt["sin"]
            B = [rt["b"]]
            dve.op(lambda e: e.tensor_scalar(out=a1[:], in0=pf[:], scalar1=rt["invf"][0:n, :], scalar2=None, op0=ALU.mult), reads=B + [b_cst], writes=B)
            dve.op(lambda e: e.tensor_scalar(out=pi_[:], in0=a1[:], scalar1=1.0 / (2 * PI), scalar2=None, op0=ALU.mult), reads=B, writes=B)
            dve.op(lambda e: e.tensor_copy(out=pf[:], in_=pi_[:]), reads=B, writes=B)
            dve.op(lambda e: e.scalar_tensor_tensor(out=a1[:], in0=pf[:], scalar=-C1, in1=a1[:], op0=ALU.mult, op1=ALU.add), reads=B, writes=B)
            dve.op(lambda e: e.scalar_tensor_tensor(out=a1[:], in0=pf[:], scalar=-C2, in1=a1[:], op0=ALU.mult, op1=ALU.add), reads=B, writes=B)
            dve.op(lambda e: e.tensor_scalar(out=pf[:], in0=a1[:], scalar1=0.0, scalar2=2 * PI, op0=ALU.is_lt, op1=ALU.mult), reads=B, writes=B)
            dve.op(lambda e: e.tensor_tensor(out=a1[:], in0=a1[:], in1=pf[:], op=ALU.add), reads=B, writes=B)
            dve.op(lambda e: e.tensor_scalar(out=a1[:], in0=a1[:], scalar1=0.0, scalar2=2 * PI, op0=ALU.max, op1=ALU.min), reads=B, writes=B)
            act.op(lambda e: e.activation(out=sin[:], in_=a1[:], func=AF.Sin, bias=rt["nb"][0:n, :], scale=rt["sgn"][0:n, :]),
                   reads=B + [b_cst], writes=[rt["bt"]])
            dve.op(lambda e: e.tensor_scalar(out=pf[:], in0=a1[:], scalar1=0.5 * PI, scalar2=None, op0=ALU.add), reads=B, writes=B)
            dve.op(lambda e: e.tensor_scalar(out=cos[:], in0=pf[:], scalar1=2 * PI, scalar2=-2 * PI, op0=ALU.is_ge, op1=ALU.mult), reads=B + [rt["bt"]], writes=B)
            dve.op(lambda e: e.tensor_tensor(out=pf[:], in0=pf[:], in1=cos[:], op=ALU.add), reads=B, writes=B)
            dve.op(lambda e: e.tensor_scalar(out=pf[:], in0=pf[:], scalar1=0.0, scalar2=2 * PI, op0=ALU.max, op1=ALU.min), reads=B, writes=B)
            act.op(lambda e: e.activation(out=cos[:], in_=pf[:], func=AF.Sin, bias=cc("pospi")[0:n, :], scale=-1.0),
                   reads=B + [b_cst], writes=[rt["bt"]])

        def proj_fm(w, wbk, c0, m, hT, bh, ntok, pbank, tsl=None):
            for k in range(8):
                rhs = hT[:, k, :] if tsl is None else hT[:, k, tsl]
                pe.op(lambda e, k=k, rhs=rhs: e.matmul(ps[pbank][0:m, 0:ntok], lhsT=w[:, k, c0:c0 + m], rhs=rhs,
                                                         start=(k == 0), stop=(k == 7)),
                      reads=[wbk, bh], writes=[pb[pbank]])

        def rope_evac(pb_x, pb_s, m, ntok, rt, tmp, btmp, dst, bdst):
            dve.op(lambda e: e.tensor_tensor(out=tmp[0:m, 0:ntok], in0=ps[pb_x][0:m, 0:ntok], in1=rt["cos"][0:m, :], op=ALU.mult),
                   reads=[pb[pb_x], rt["bt"]], writes=[btmp])
            dve.op(lambda e: e.tensor_tensor(out=tmp[0:m, 512:512 + ntok], in0=ps[pb_s][0:m, 0:ntok], in1=rt["sin"][0:m, :], op=ALU.mult),
                   reads=[pb[pb_s], rt["bt"]], writes=[btmp])
            pool.op(lambda e: e.tensor_tensor(out=dst, in0=tmp[0:m, 0:ntok], in1=tmp[0:m, 512:512 + ntok], op=ALU.add),
                    reads=[btmp], writes=[bdst])

        b_dram = Buf()
        with ExitStack() as st1:
            w1, w1b = load_w(wext(C_W1, 1024), 8, 1024)
            w1v, w1vb = load_w(wext(C_W1 + 832, 512), 8, 512)
            hh = [make_h(st1, f"p1h{i}", None, 512) for i in range(2)]
            rtA = [rope_tables(st1, "p1ra", None, 512, 128, cc("invfA"), cc("sgnA"), cc("nbA"))] * 2
            rtI = [rope_tables(st1, "p1ri", None, 512, 32, cc("invfI"), cc("sgnI"), cc("nbI"))] * 2
            tmp = T("p1tmp", [128, 1024], F32, st1)
            btmp = Buf()
            kst = [T(f"p1k{i}", [128, 5, 512], BF16, st1) for i in range(2)]
            bk = [Buf(), Buf()]
            vst = [T(f"p1v{i}", [128, 4, 512], BF16, st1) for i in range(2)]
            bv = [Buf(), Buf()]
            osem = [DSem(K, "p1o0"), DSem(K, "p1o1")]
            for ch in range(P1_CHUNKS):
                i = ch % 2
                t0 = ch * 512
                run_h(hh[i], lambda k, t0=t0: xT_full[k * 128:(k + 1) * 128, t0:t0 + 512], 7)
                run_rope_tables(rtA[i], pos_full[:, t0:t0 + 512])
                run_rope_tables(rtI[i], pos_full[:, t0:t0 + 512])
                hT, bh = hh[i]["hT"], hh[i]["bh"]
                for pr in range(2):
                    proj_fm(w1, w1b, pr * 128, 128, hT, bh, 512, 0)
                    proj_fm(w1, w1b, 256 + pr * 128, 128, hT, bh, 512, 1)
                    rope_evac(0, 1, 128, 512, rtA[i], tmp, btmp, kst[i][:, pr, :], bk[i])
                for pr in range(2):
                    proj_fm(w1, w1b, 512 + pr * 128, 128, hT, bh, 512, 2 + pr)
                    act.op(lambda e, pr=pr: e.copy(out=kst[i][:, 2 + pr, :], in_=ps[2 + pr][:, :]), reads=[pb[2 + pr]], writes=[bk[i]])
                proj_fm(w1, w1b, 768, 32, hT, bh, 512, 0)
                proj_fm(w1, w1b, 800, 32, hT, bh, 512, 1)
                rope_evac(0, 1, 32, 512, rtI[i], tmp, btmp, kst[i][0:32, 4, :], bk[i])
                for bl in range(4):
                    pbk = 4 + (bl % 2)
                    for k in range(8):
                        pe.op(lambda e, k=k, bl=bl, pbk=pbk: e.matmul(ps[pbk][:, :], lhsT=hT[:, k, bl * 128:(bl + 1) * 128], rhs=w1v[:, k, 0:512],
                                                                       start=(k == 0), stop=(k == 7)), reads=[bh, w1vb], writes=[pb[pbk]])
                    act.op(lambda e, bl=bl, pbk=pbk: e.copy(out=vst[i][:, bl, :], in_=ps[pbk][:, :]), reads=[pb[pbk]], writes=[bv[i]])
                for pr in range(2):
                    sp.dma(KTA[pr, :, t0:t0 + 512], kst[i][:, pr, :], osem[i], reads=[bk[i]] if pr == 0 else [], writes=[b_dram] if pr == 0 else [])
                    sp.dma(KTC[pr, :, t0:t0 + 512], kst[i][:, 2 + pr, :], osem[i])
                sp.dma(IKT[:, t0:t0 + 512], kst[i][0:32, 4, :], osem[i])
                sp.dma(VAC[t0:t0 + 512, :].rearrange("(b p) c -> p b c", p=128), vst[i][:], osem[i], reads=[bv[i]])
                tk = (osem[i].h, osem[i].cnt, None)
                bk[i].r[osem[i]] = tk
                bv[i].r[osem[i]] = tk
                b_dram.w = tk
            wB, wBb = load_w(wext(C_WB, 1024), 8, 1024)
            hhal = make_h(st1, "hal", None, 32)
            run_h(hhal, lambda k: xT_halo[k * 128:(k + 1) * 128, :], 7)
            bhal = Buf()
            for tl in range(2):
                proj_fm(wB, wBb, 256 + tl * 128, 128, hhal["hT"], hhal["bh"], 32, 0)
                proj_fm(wB, wBb, 512 + tl * 128, 128, hhal["hT"], hhal["bh"], 32, 1)
                act.op(lambda e: e.copy(out=tmp[:, 0:32], in_=ps[0][:, 0:32]), reads=[pb[0]], writes=[btmp])
                dve.op(lambda e, tl=tl: e.tensor_tensor(out=yhalo[:, tl, :], in0=ps[1][:, 0:32], in1=tmp[:, 0:32], op=ALU.mult),
                       reads=[pb[1], btmp], writes=[bhal])
            K.barrier()

        kvs = [DSem(K, f"kv{i}") for i in range(3)]
        kt = [T(f"kt{i}", [128, 2, 512], BF16) for i in range(3)]
        vt = [T(f"vt{i}", [128, 4, 256], BF16) for i in range(3)]
        ikt = [T(f"ikt{i}", [32, 512], BF16) for i in range(3)]
        kvb = [Buf() for _ in range(3)]
        kvctr = [0]

        def load_kv(which, c0):
            i = kvctr[0] % 3
            kvctr[0] += 1
            if which == "I":
                sp.dma(ikt[i][:], IKT[:, c0:c0 + 512], kvs[i], writes=[kvb[i]])
            else:
                KT = KTA if which == "A" else KTC
                vo = 0 if which == "A" else 256
                sp.dma(kt[i][:], KT[:, :, c0:c0 + 512].rearrange("a p t -> p a t"), kvs[i], writes=[kvb[i]])
                sp.dma(vt[i][:], VAC[c0:c0 + 512, vo:vo + 256].rearrange("(b p) c -> p b c", p=128), kvs[i])
                kvb[i].w = (kvs[i].h, kvs[i].cnt, None)
            return i

        osem2 = DSem(K, "out")
        b_out = Buf()
        for gi in range(NGROUPS):
            tg0 = gi * 512
            K.mute = False
            with ExitStack() as sg:
                with ExitStack() as sth:
                    hg = make_h(sg, "hg", None, 512, tst=sth)
                    run_h(hg, lambda k: xT_own[k * 128:(k + 1) * 128, tg0:tg0 + 512], 7)
                    K.barrier()
                hT, bh = hg["hT"], hg["bh"]
                oT = T("oT", [128, 4, 2, 512], BF16, sg)
                boT = Buf()
                gsl = T("gsl", [128, 8, 512], BF16, sg)
                bgs = Buf()
                pTb = T("pTb", [128, 2, 512], BF16, sg)
                bpT = Buf()
                dpt = DSem(K, "dpt")
                for k in range(2):
                    pool.dma(pTb[:, k, :], pT_own[k * 128:(k + 1) * 128, tg0:tg0 + 512], dpt, writes=[bpT] if k == 0 else [])
                bpT.w = (dpt.h, dpt.cnt, None)
                qA = T("qA", [128, 2, 512], BF16, sg)
                qC = T("qC", [128, 2, 512], BF16, sg)
                qAz = T("qAz", [128, 4, 512], BF16, sg)
                qCz = T("qCz", [128, 4, 512], BF16, sg)
                iq = T("iq", [32, 4, 512], BF16, sg)
                wq = T("wq", [128, 4, 4], F32, sg)
                bq = Buf()
                K.mute = ("gate" not in STAGES)
                wD, wDb = load_w(wext(C_WD, 1024), 8, 1024)
                for tl in range(8):
                    pbk = tl % 2
                    proj_fm(wD, wDb, tl * 128, 128, hT, bh, 512, pbk)
                    act.op(lambda e, tl=tl, pbk=pbk: e.activation(out=gsl[:, tl, :], in_=ps[pbk][:, :], func=AF.Silu),
                           reads=[pb[pbk]], writes=[bgs])
                K.mute = ("bd" not in STAGES)
                with ExitStack() as sbd:
                    wB, wBb = load_w(wext(C_WB, 1024), 8, 1024)
                    bbt = T("bbt", [128, 2, 512], F32, sbd)
                    ypad = T("ypad", [128, 2, 4, 130], F32, sbd)
                    acc = T("acc", [128, 2, 512], F32, sbd)
                    ut = T("ut", [128, 2, 512], F32, sbd)
                    bbd = Buf()
                    tmpb = T("tmpb", [128, 512], F32, sbd)
                    btb = Buf()
                    for tl in range(2):
                        proj_fm(wB, wBb, tl * 128, 128, hT, bh, 512, 0)
                        act.op(lambda e, tl=tl: e.copy(out=bbt[:, tl, :], in_=ps[0][:, :]), reads=[pb[0]], writes=[bbd])
                        proj_fm(wB, wBb, 256 + tl * 128, 128, hT, bh, 512, 1)
                        proj_fm(wB, wBb, 512 + tl * 128, 128, hT, bh, 512, 2)
                        act.op(lambda e: e.copy(out=tmpb[:], in_=ps[1][:, :]), reads=[pb[1]], writes=[btb])
                        dve.op(lambda e, tl=tl: e.tensor_tensor(out=ypad[:, tl, :, 2:130], in0=ps[2][:, :].rearrange("p (b t) -> p b t", b=4),
                                                                in1=tmpb[:].rearrange("p (b t) -> p b t", b=4), op=ALU.mult),
                               reads=[pb[2], btb], writes=[bbd])
                        pool.op(lambda e, tl=tl: e.tensor_copy(out=ypad[:, tl, :, 0:2],
                                                               in_=yhalo[:, tl, gi * 8:gi * 8 + 8].rearrange("p (b t) -> p b t", b=4)),
                                reads=[bhal], writes=[bbd])
                        proj_fm(wB, wBb, 768 + tl * 128, 128, hT, bh, 512, 3)
                        act.op(lambda e, tl=tl: e.copy(out=ut[:, tl, :], in_=ps[3][:, :]), reads=[pb[3]], writes=[bbd])
                    for tl in range(2):
                        a3 = acc[:, tl, :].rearrange("p (b t) -> p b t", b=4)
                        pool.op(lambda e, tl=tl, a3=a3: e.tensor_scalar(out=a3, in0=ypad[:, tl, :, 0:128], scalar1=cw[:, tl * 3:tl * 3 + 1],
                                                                         scalar2=None, op0=ALU.mult), reads=[bbd, b_vec], writes=[bbd])
                        for kk in (1, 2):
                            dve.op(lambda e, tl=tl, a3=a3, kk=kk: e.scalar_tensor_tensor(out=a3, in0=ypad[:, tl, :, kk:kk + 128],
                                                                                         scalar=cw[:, tl * 3 + kk:tl * 3 + kk + 1], in1=a3,
                                                                                         op0=ALU.mult, op1=ALU.add), reads=[bbd, b_vec], writes=[bbd])
                        dve.op(lambda e, tl=tl: e.scalar_tensor_tensor(out=acc[:, tl, :], in0=acc[:, tl, :], scalar=cb[:, tl:tl + 1], in1=bbt[:, tl, :],
                                                                        op0=ALU.add, op1=ALU.mult), reads=[bbd, b_vec], writes=[bbd])
                        pool.op(lambda e, tl=tl: e.tensor_tensor(out=oT[:, 1, tl, :], in0=acc[:, tl, :], in1=gsl[:, 2 + tl, :], op=ALU.mult),
                                reads=[bbd, bgs], writes=[boT])
                    K.mute = ("d" not in STAGES)
                    wC_, wCb = load_w(wext(C_WC, 260), 8, 260)
                    vn = T("vn", [128, 256], F32, sbd)
                    vnz = T("vnz", [128, 4, 128], BF16, sbd)
                    st4 = T("st4", [128, 8], F32, sbd)
                    junk = T("junkd", [128, 256], F32, sbd)
                    bvn, bst = Buf(), Buf()
                    pool.op(lambda e: e.memset(vnz[:], 0.0), writes=[bvn])
                    for bl in range(4):
                        tsl = slice(bl * 128, (bl + 1) * 128)
                        for k in range(8):
                            pe.op(lambda e, k=k, tsl=tsl: e.matmul(ps[4][:, 0:260], lhsT=hT[:, k, tsl], rhs=wC_[:, k, 0:260], start=(k == 0), stop=(k == 7)),
                                  reads=[bh, wCb], writes=[pb[4]])
                        dve.op(lambda e, bl=bl: e.tensor_scalar(out=wq[:, bl, :], in0=ps[4][:, 256:260], scalar1=IDX_W_SCALE, scalar2=None, op0=ALU.mult),
                               reads=[pb[4]], writes=[bq])
                        act.op(lambda e: e.activation(out=vn[:], in_=ps[4][:, 0:256], func=AF.Copy, accum_out=st4[:, 0:1]), reads=[pb[4]], writes=[bvn, bst])
                        act.op(lambda e: e.activation(out=junk[:], in_=ps[4][:, 0:256], func=AF.Square, accum_out=st4[:, 1:2]), reads=[pb[4]], writes=[bst])
                        dve.op(lambda e: e.tensor_scalar(out=st4[:, 2:3], in0=st4[:, 0:1], scalar1=1.0 / 256, scalar2=None, op0=ALU.mult), reads=[bst], writes=[bst])
                        dve.op(lambda e: e.tensor_tensor(out=st4[:, 3:4], in0=st4[:, 2:3], in1=st4[:, 2:3], op=ALU.mult), reads=[bst], writes=[bst])
                        dve.op(lambda e: e.scalar_tensor_tensor(out=st4[:, 4:5], in0=st4[:, 1:2], scalar=1.0 / 256, in1=st4[:, 3:4], op0=ALU.mult, op1=ALU.subtract),
                               reads=[bst], writes=[bst])
                        act.op(lambda e: e.activation(out=st4[:, 5:6], in_=st4[:, 4:5], func=AF.Ln, bias=cc("eps"), scale=1.0), reads=[bst, b_cst], writes=[bst])
                        act.op(lambda e: e.activation(out=st4[:, 5:6], in_=st4[:, 5:6], func=AF.Exp, scale=-0.5), reads=[bst], writes=[bst])
                        dve.op(lambda e: e.tensor_scalar(out=vn[:], in0=vn[:], scalar1=st4[:, 2:3], scalar2=st4[:, 5:6], op0=ALU.subtract, op1=ALU.mult),
                               reads=[bvn, bst], writes=[bvn])
                        dve.op(lambda e: e.tensor_tensor(out=vn[:], in0=vn[:], in1=lnbt[:, 0:256], op=ALU.mult), reads=[bvn, b_vec], writes=[bvn])
                        for g in range(4):
                            dve.op(lambda e, g=g: e.tensor_tensor(out=vnz[:, g, (g % 2) * 64:(g % 2) * 64 + 64], in0=vn[:, g * 64:(g + 1) * 64],
                                                                  in1=lnbt[:, 256 + g * 64:256 + (g + 1) * 64], op=ALU.add), reads=[bvn, b_vec], writes=[bvn])
                        for pr in range(2):
                            for gg in range(2):
                                g = 2 * pr + gg
                                pe.op(lambda e, g=g, gg=gg: e.matmul(ps[5][:, 0:128], lhsT=vnz[:, g, :], rhs=wst[:, g * 128:(g + 1) * 128], start=(gg == 0), stop=False),
                                      reads=[bvn, b_vec], writes=[pb[5]])
                            pe.op(lambda e, pr=pr: e.matmul(ps[5][:, 0:128], lhsT=egb[0:4, pr * 128:(pr + 1) * 128], rhs=bspb[0:4, :], start=False, stop=True),
                                  reads=[b_cst, b_vec], writes=[pb[5]])
                            dve.op(lambda e, pr=pr, tsl=tsl: e.tensor_tensor(out=ut[:, pr, tsl], in0=ps[5][:, 0:128], in1=ut[:, pr, tsl], op=ALU.mult),
                                   reads=[pb[5], bbd], writes=[bbd])
                            pool.op(lambda e, pr=pr, tsl=tsl: e.tensor_tensor(out=oT[:, 3, pr, tsl], in0=ut[:, pr, tsl], in1=gsl[:, 6 + pr, tsl], op=ALU.mult),
                                    reads=[bbd, bgs], writes=[boT])
                    K.barrier()

                K.mute = ("q" not in STAGES)
                with ExitStack() as sat:
                    rtA = rope_tables(sat, "rqa", None, 512, 128, cc("invfA"), cc("sgnA"), cc("nbA"))
                    rtI = rope_tables(sat, "rqi", None, 512, 32, cc("invfI"), cc("sgnI"), cc("nbI"))
                    run_rope_tables(rtA, pos_own[:, tg0:tg0 + 512])
                    run_rope_tables(rtI, pos_own[:, tg0:tg0 + 512])
                    tmp = T("tmpq", [128, 1024], F32, sat)
                    btmp = Buf()
                    wA, wAb = load_w(wext(C_WA, 1024), 8, 1024)
                    pool.op(lambda e: e.memset(qAz[:], 0.0), writes=[bq])
                    pool.op(lambda e: e.memset(qCz[:], 0.0), writes=[bq])
                    for pr in range(2):
                        proj_fm(wA, wAb, pr * 128, 128, hT, bh, 512, 0)
                        proj_fm(wA, wAb, 256 + pr * 128, 128, hT, bh, 512, 1)
                        rope_evac(0, 1, 128, 512, rtA, tmp, btmp, qA[:, pr, :], bq)
                        pool.op(lambda e, pr=pr: e.tensor_copy(out=qAz[0:64, 2 * pr, :], in_=qA[0:64, pr, :]), reads=[bq], writes=[bq])
                        pool.op(lambda e, pr=pr: e.tensor_copy(out=qAz[64:128, 2 * pr + 1, :], in_=qA[64:128, pr, :]), reads=[bq], writes=[bq])
                        proj_fm(wA, wAb, 512 + pr * 128, 128, hT, bh, 512, 2)
                        act.op(lambda e, pr=pr: e.copy(out=qC[:, pr, :], in_=ps[2][:, :]), reads=[pb[2]], writes=[bq])
                        pool.op(lambda e, pr=pr: e.tensor_copy(out=qCz[0:64, 2 * pr, :], in_=qC[0:64, pr, :]), reads=[bq], writes=[bq])
                        pool.op(lambda e, pr=pr: e.tensor_copy(out=qCz[64:128, 2 * pr + 1, :], in_=qC[64:128, pr, :]), reads=[bq], writes=[bq])
                    for h in range(4):
                        proj_fm(wA, wAb, 768 + h * 32, 32, hT, bh, 512, 0)
                        proj_fm(wA, wAb, 896 + h * 32, 32, hT, bh, 512, 1)
                        rope_evac(0, 1, 32, 512, rtI, tmp, btmp, iq[0:32, h, :], bq)
                    K.barrier()

                with ExitStack() as sa:
                    Sc = T("Sc", [128, S], F32, sa)
                    bS = Buf()
                    junk = T("junk", [128, 2048], BF16, sa)
                    rl = [T(f"rl{i}", [128, 512], F32, sa) for i in range(2)]
                    brl = [Buf(), Buf()]
                    bis = T("bis", [128, 64], F32, sa)
                    bbis = Buf()
                    Et = [T(f"Et{i}", [128, 512], F32, sa) for i in range(3)]
                    Lt = [T(f"Lt{i}", [128, 512], BF16, sa) for i in range(3)]
                    Xt = [T(f"Xt{i}", [128, 512], F32, sa) for i in range(3)]
                    Wt = [T(f"Wt{i}", [128, 512], BF16, sa) for i in range(3)]
                    bE = [Buf(), Buf(), Buf()]
                    bL = [Buf(), Buf(), Buf()]
                    bX = [Buf(), Buf(), Buf()]
                    bW = [Buf(), Buf(), Buf()]
                    car = T("car", [32, 1024], BF16, sa)
                    bcar = Buf()
                    fin = T("fin", [128, 512], F32, sa)
                    bfin = Buf()
                    identf = cc("ident")

                    def p1(bl):
                        l = gi * 4 + bl
                        NK = (8 * l + 8) * 128
                        nkb = 8 * l + 8
                        tsl = slice(bl * 128, (bl + 1) * 128)
                        K.mute = ("score" not in STAGES)
                        for c0 in range(0, NK, 512):
                            ki = load_kv("I", c0)
                            for h in range(4):
                                pbk = h % 2
                                pe.op(lambda e, h=h, pbk=pbk, ki=ki: e.matmul(ps[pbk][:, :], lhsT=iq[0:32, h, tsl], rhs=ikt[ki][0:32, :], start=True, stop=True),
                                      reads=[bq, kvb[ki]], writes=[pb[pbk]])
                                act.op(lambda e, pbk=pbk: e.activation(out=rl[pbk][:], in_=ps[pbk][:, :], func=AF.Relu), reads=[pb[pbk]], writes=[brl[pbk]])
                                if h == 0:
                                    dve.op(lambda e, c0=c0, pbk=pbk: e.tensor_scalar(out=Sc[:, c0:c0 + 512], in0=rl[pbk][:], scalar1=wq[:, bl, 0:1], scalar2=None, op0=ALU.mult),
                                           reads=[brl[pbk], bq], writes=[bS])
                                else:
                                    dve.op(lambda e, c0=c0, pbk=pbk, h=h: e.scalar_tensor_tensor(out=Sc[:, c0:c0 + 512], in0=rl[pbk][:], scalar=wq[:, bl, h:h + 1],
                                                                                                 in1=Sc[:, c0:c0 + 512], op0=ALU.mult, op1=ALU.add),
                                           reads=[brl[pbk], bq], writes=[bS])
                        K.mute = ("bis" not in STAGES)
                        tail = Sc[:, NK - 1024:NK]
                        mn, mx, mn2, lo, w0, cnt, dd, mid = (bis[:, i:i + 1] for i in range(8))
                        hwt = bis[:, 8:8 + NIT + 1]
                        cnts = bis[:, 40:48]
                        dve.op(lambda e: e.tensor_tensor(out=fin[:, 0:512], in0=tail[:, 0:512], in1=cmt[:, 0:512], op=ALU.subtract), reads=[bS, b_vec], writes=[bfin])
                        dve.op(lambda e: e.tensor_reduce(out=mn, in_=fin[:, 0:512], axis=AX.X, op=ALU.min), reads=[bfin], writes=[bbis])
                        dve.op(lambda e: e.tensor_tensor(out=fin[:, 0:512], in0=tail[:, 512:1024], in1=cmt[:, 512:1024], op=ALU.subtract), reads=[bS, b_vec, bbis], writes=[bfin])
                        dve.op(lambda e: e.tensor_reduce(out=mn2, in_=fin[:, 0:512], axis=AX.X, op=ALU.min), reads=[bfin], writes=[bbis])
                        dve.op(lambda e: e.tensor_tensor(out=mn, in0=mn, in1=mn2, op=ALU.min), reads=[bbis], writes=[bbis])
                        if NK > 1024:
                            dve.op(lambda e: e.tensor_reduce(out=mn2, in_=Sc[:, 0:NK - 1024], axis=AX.X, op=ALU.min), reads=[bS, bbis], writes=[bbis])
                            dve.op(lambda e: e.tensor_tensor(out=mn, in0=mn, in1=mn2, op=ALU.min), reads=[bbis], writes=[bbis])
                        dve.op(lambda e: e.tensor_tensor(out=tail, in0=tail, in1=cmt[:], op=ALU.add), reads=[bS, b_vec], writes=[bS])
                        dve.op(lambda e: e.tensor_reduce(out=mx, in_=Sc[:, 0:NK], axis=AX.X, op=ALU.max), reads=[bS, bbis], writes=[bbis])
                        dve.op(lambda e: e.tensor_tensor(out=w0, in0=mx, in1=mn, op=ALU.subtract), reads=[bbis], writes=[bbis])
                        dve.op(lambda e: e.tensor_scalar(out=w0, in0=w0, scalar1=1.001, scalar2=1e-20, op0=ALU.mult, op1=ALU.add), reads=[bbis], writes=[bbis])
                        dve.op(lambda e: e.tensor_scalar(out=hwt, in0=cc("hw"), scalar1=w0, scalar2=None, op0=ALU.mult), reads=[bbis, b_cst], writes=[bbis])
                        dve.op(lambda e: e.tensor_tensor(out=mid, in0=mn, in1=hwt[:, 0:1], op=ALU.add), reads=[bbis], writes=[bbis])
                        dve.op(lambda e: e.tensor_copy(out=lo, in_=mn), reads=[bbis], writes=[bbis])
                        nch = (NK + 2047) // 2048
                        for it in range(NIT):
                            for ci in range(nch):
                                a = ci * 2048
                                wdt = min(2048, NK - a)
                                dve.op(lambda e, a=a, wdt=wdt, ci=ci: e.tensor_scalar(out=junk[:, 0:wdt], in0=Sc[:, a:a + wdt], scalar1=mid, scalar2=0.0,
                                                                                      op0=ALU.is_ge, op1=ALU.add, accum_out=cnts[:, ci:ci + 1]),
                                       reads=[bS, bbis] if ci == 0 else [bS], writes=[])
                            bbis.w = dve.last
                            if nch > 1:
                                dve.op(lambda e: e.tensor_reduce(out=cnt, in_=cnts[:, 0:nch], axis=AX.X, op=ALU.add), reads=[bbis], writes=[bbis])
                                cn = cnt
                            else:
                                cn = cnts[:, 0:1]
                            dve.op(lambda e, it=it, cn=cn: e.tensor_scalar(out=dd, in0=cn, scalar1=TOPK, scalar2=hwt[:, it:it + 1], op0=ALU.is_ge, op1=ALU.mult),
                                   reads=[bbis], writes=[bbis])
                            dve.op(lambda e: e.tensor_tensor(out=lo, in0=lo, in1=dd, op=ALU.add), reads=[bbis], writes=[bbis])
                            dve.op(lambda e, it=it: e.tensor_tensor(out=mid, in0=lo, in1=hwt[:, it + 1:it + 2], op=ALU.add), reads=[bbis], writes=[bbis])
                        for a in range(0, NK, 4096):
                            wdt = min(4096, NK - a)
                            dve.op(lambda e, a=a, wdt=wdt: e.tensor_scalar(out=Sc[:, a:a + wdt], in0=Sc[:, a:a + wdt], scalar1=lo, scalar2=None, op0=ALU.subtract),
                                    reads=[bS, bbis], writes=[bS])
                    def pA(bl):
                        QB = [0, 1, 6]
                        TB = [2, 4, 5]
                        l = gi * 4 + bl
                        NK = (8 * l + 8) * 128
                        nkb = 8 * l + 8
                        tsl = slice(bl * 128, (bl + 1) * 128)
                        K.mute = ("A" not in STAGES)
                        dve.op(lambda e: e.memset(ps[3][:, :], 0.0), writes=[pb[3]])
                        kis = {}

                        def s1(kb):
                            c0, kb4 = (kb // 4) * 512, kb % 4
                            if c0 not in kis:
                                kis[c0] = load_kv("A", c0)
                            ki = kis[c0]
                            j = kb % 3
                            ksl = slice(kb4 * 128, (kb4 + 1) * 128)
                            pe.op(lambda e: e.matmul(ps[TB[j]][:, 0:128], lhsT=Sc[:, kb * 128:(kb + 1) * 128], rhs=identf, start=True, stop=True),
                                  reads=[bS, b_cst], writes=[pb[TB[j]]])
                            for h in range(4):
                                pr = h // 2
                                pe.op(lambda e: e.matmul(ps[QB[j]][:, h * 128:(h + 1) * 128], lhsT=kt[ki][:, pr, ksl], rhs=qAz[:, h, tsl], start=True, stop=True),
                                      reads=[kvb[ki], bq], writes=[pb[QB[j]]])
                            act.op(lambda e: e.activation(out=Wt[j][:], in_=ps[QB[j]][:, :], func=AF.Exp, scale=0.125), reads=[pb[QB[j]]], writes=[bW[j]])

                        def s2(kb):
                            c0, kb4 = (kb // 4) * 512, kb % 4
                            ki = kis[c0]
                            j = kb % 3
                            dve.op(lambda e: e.scalar_tensor_tensor(out=Lt[j][:].rearrange("p (h t) -> p h t", h=4),
                                                                    in0=ps[TB[j]][:, 0:128].unsqueeze(1).to_broadcast([128, 4, 128]), scalar=0.0,
                                                                    in1=Wt[j][:].rearrange("p (h t) -> p h t", h=4), op0=ALU.is_ge, op1=ALU.mult),
                                   reads=[pb[TB[j]], bW[j]], writes=[bL[j]])
                            for h in range(4):
                                pr, off = h // 2, 64 * (h % 2)
                                pe.op(lambda e: e.matmul(ps[3][off:off + 64, pr * 128:(pr + 1) * 128], lhsT=vt[ki][:, kb4, h * 64:(h + 1) * 64],
                                                         rhs=Lt[j][:, h * 128:(h + 1) * 128], start=False, stop=(kb == nkb - 1)),
                                      reads=[kvb[ki], bL[j]], writes=[pb[3]])
                                pe.op(lambda e: e.matmul(ps[3][off:off + 64, 256 + pr * 128:256 + (pr + 1) * 128], lhsT=onesb[:, 0:64],
                                                         rhs=Lt[j][:, h * 128:(h + 1) * 128], start=False, stop=(kb == nkb - 1)),
                                      reads=[b_cst, bL[j]], writes=[pb[3]])

                        for step in range(nkb + 1):
                            if step < nkb:
                                s1(step)
                            if step >= 1:
                                s2(step - 1)
                        dve.op(lambda e: e.reciprocal(out=fin[:, 0:256], in_=ps[3][:, 256:512]), reads=[pb[3]], writes=[bfin])
                        dve.op(lambda e: e.tensor_tensor(out=fin[:, 256:512], in0=ps[3][:, 0:256], in1=fin[:, 0:256], op=ALU.mult), reads=[pb[3], bfin], writes=[bfin])
                        pool.op(lambda e: e.tensor_tensor(out=oT[:, 0, :, tsl], in0=fin[:, 256:512].rearrange("p (a t) -> p a t", a=2), in1=gsl[:, 0:2, tsl], op=ALU.mult),
                                reads=[bfin, bgs], writes=[boT])

                    def pC(bl):
                        QB = [0, 1, 6]
                        CB = [4, 5, 7]
                        l = gi * 4 + bl
                        NK = (8 * l + 8) * 128
                        tsl = slice(bl * 128, (bl + 1) * 128)
                        K.mute = ("C" not in STAGES)
                        its = [(c0, kb4) for c0 in range(NK - 512, -1, -512) for kb4 in range(3, -1, -1)]
                        n = len(its)
                        kis = {}

                        def s1(idx):
                            c0, kb4 = its[idx]
                            if c0 not in kis:
                                kis[c0] = load_kv("C", c0)
                            ki = kis[c0]
                            kb = c0 // 128 + kb4
                            j = idx % 3
                            o = kb - 8 * l
                            ksl = slice(kb4 * 128, (kb4 + 1) * 128)
                            for h in range(4):
                                pr = h // 2
                                pe.op(lambda e: e.matmul(ps[QB[j]][:, h * 128:(h + 1) * 128], lhsT=kt[ki][:, pr, ksl], rhs=qCz[:, h, tsl], start=True, stop=True),
                                      reads=[kvb[ki], bq], writes=[pb[QB[j]]])
                            act.op(lambda e: e.activation(out=Et[j][:], in_=ps[QB[j]][:, :], func=AF.Exp, scale=0.125), reads=[pb[QB[j]]], writes=[bE[j]])
                            act.op(lambda e: e.activation(out=Lt[j][:], in_=Et[j][:], func=AF.Ln, bias=cc("one"), scale=1.0), reads=[bE[j], b_cst], writes=[bL[j]])
                            if o >= 0:
                                for hh_ in range(4):
                                    hs = slice(hh_ * 128, (hh_ + 1) * 128)
                                    pool.op(lambda e: e.tensor_tensor(out=Et[j][:, hs], in0=Et[j][:, hs], in1=mst[:, o * 128:(o + 1) * 128], op=ALU.mult),
                                            reads=[bE[j], b_vec], writes=[bE[j]])
                                    pool.op(lambda e: e.tensor_tensor(out=Lt[j][:, hs], in0=Lt[j][:, hs], in1=mst[:, o * 128:(o + 1) * 128], op=ALU.mult),
                                            reads=[bL[j], b_vec], writes=[bL[j]])

                        def s2(idx):
                            c0, kb4 = its[idx]
                            ki = kis[c0]
                            kb = c0 // 128 + kb4
                            j = idx % 3
                            first = (idx == 0)
                            pe.op(lambda e: e.matmul(ps[CB[j]][:, :], lhsT=trib, rhs=Lt[j][:], start=True, stop=first), reads=[b_cst, bL[j]], writes=[pb[CB[j]]])
                            if not first:
                                pe.op(lambda e: e.matmul(ps[CB[j]][:, :], lhsT=onesb[0:1, :], rhs=car[0:1, 0:512], start=False, stop=True), reads=[b_cst, bcar], writes=[pb[CB[j]]])
                            if kb > 0:
                                act.op(lambda e: e.copy(out=car[0:32, 0:512], in_=ps[CB[j]][0:32, :]), reads=[pb[CB[j]]], writes=[bcar])
                            act.op(lambda e: e.activation(out=Xt[j][:], in_=ps[CB[j]][:, :], func=AF.Exp, scale=-1.0), reads=[pb[CB[j]]], writes=[bX[j]])
                            pool.op(lambda e: e.tensor_tensor(out=Wt[j][:], in0=Et[j][:], in1=Xt[j][:], op=ALU.mult), reads=[bE[j], bX[j]], writes=[bW[j]])
                            for h in range(4):
                                pr, off = h // 2, 64 * (h % 2)
                                pe.op(lambda e: e.matmul(ps[3][off:off + 64, pr * 128:(pr + 1) * 128], lhsT=vt[ki][:, kb4, h * 64:(h + 1) * 64],
                                                         rhs=Wt[j][:, h * 128:(h + 1) * 128], start=False, stop=(kb == 0)),
                                      reads=[kvb[ki], bW[j]], writes=[pb[3]])

                        for step in range(n + 1):
                            if step < n:
                                s1(step)
                            if step >= 1:
                                s2(step - 1)
                        dve.op(lambda e: e.tensor_tensor(out=oT[:, 2, :, tsl], in0=ps[3][:, 0:256].rearrange("p (a t) -> p a t", a=2), in1=gsl[:, 4:6, tsl], op=ALU.mult),
                               reads=[pb[3], bgs], writes=[boT])

                    p1(0)
                    for bl in range(4):
                        pA(bl)
                        K.mute = ("C" not in STAGES)
                        dve.op(lambda e: e.memset(ps[3][:, :], 0.0), writes=[pb[3]])
                        if bl < 3:
                            p1(bl + 1)
                        pC(bl)
                    K.barrier()

                K.mute = ("post" not in STAGES)
                with ExitStack() as spo:
                    xg = [T(f"xg{i}", [128, 512], F32, spo) for i in range(2)]
                    bxg = [Buf(), Buf()]
                    dxg = [DSem(K, "dxg0"), DSem(K, "dxg1")]
                    mg = T("mg", [128, 8, 512], F32, spo)
                    mgb = T("mgb", [128, 8, 512], BF16, spo)
                    yt = T("yt", [128, 8, 512], F32, spo)
                    ysq = [T(f"ysq{i}", [128, 512], BF16, spo) for i in range(2)]
                    gt = [T(f"gt{i}", [128, 512], F32, spo) for i in range(2)]
                    tt = [T(f"tt{i}", [128, 512], F32, spo) for i in range(2)]
                    r2 = T("r2", [128, 512], F32, spo)
                    bmg_, bmgb, byt, br2 = Buf(), Buf(), Buf(), Buf()
                    bysq = [Buf(), Buf()]
                    bgt = [Buf(), Buf()]
                    btt = [Buf(), Buf()]
                    wbr = T("wbr", [128, 8, 1024], BF16, spo)
                    wbrb = Buf()
                    dwbr = DSem(K, "dwbr")
                    for k in range(8):
                        pool.dma(wbr[:, k, :], w_branch[k // 2, (k % 2) * 128:(k % 2 + 1) * 128, :], dwbr, writes=[wbrb] if k == 0 else [])
                    wbrb.w = (dwbr.h, dwbr.cnt, None)
                    q = 0
                    for n in range(4):
                        wm, wmb = load_w(wext(C_WM + n * 1024, 1024), 8, 1024)
                        for dt_ in range(8):
                            j = q % 2
                            q += 1
                            for kk in range(2):
                                pe.op(lambda e, kk=kk, j=j, n=n, dt_=dt_: e.matmul(ps[j][:, :], lhsT=wbr[:, n * 2 + kk, dt_ * 128:(dt_ + 1) * 128], rhs=oT[:, n, kk, :],
                                                                                  start=(kk == 0), stop=(kk == 1)), reads=[wbrb, boT], writes=[pb[j]])
                            proj_fm(wm, wmb, dt_ * 128, 128, hT, bh, 512, 2 + j)
                            act.op(lambda e, j=j, n=n, dt_=dt_: e.activation(out=gt[j][:], in_=ps[2 + j][:, :], func=AF.Sigmoid, bias=bmg[:, n * 8 + dt_:n * 8 + dt_ + 1], scale=1.0),
                                   reads=[pb[2 + j], b_vec], writes=[bgt[j]])
                            if n == 0:
                                dve.op(lambda e, j=j, dt_=dt_: e.tensor_tensor(out=mg[:, dt_, :], in0=ps[j][:, :], in1=gt[j][:], op=ALU.mult), reads=[pb[j], bgt[j]], writes=[bmg_])
                            else:
                                dve.op(lambda e, j=j: e.tensor_tensor(out=tt[j][:], in0=ps[j][:, :], in1=gt[j][:], op=ALU.mult), reads=[pb[j], bgt[j]], writes=[btt[j]])
                                pool.op(lambda e, j=j, dt_=dt_: e.tensor_tensor(out=mg[:, dt_, :], in0=mg[:, dt_, :], in1=tt[j][:], op=ALU.add), reads=[btt[j], bmg_], writes=[bmg_])
                    pool.op(lambda e: e.tensor_copy(out=mgb[:], in_=mg[:]), reads=[bmg_], writes=[bmgb])
                    wo, wob = load_w(lambda k: w_out[k * 128:(k + 1) * 128, :], 8, 1024)
                    for dt_ in range(8):
                        j = dt_ % 2
                        proj_fm(wo, wob, dt_ * 128, 128, mgb, bmgb, 512, j)
                        act.op(lambda e, j=j, dt_=dt_: e.copy(out=yt[:, dt_, :], in_=ps[j][:, :]), reads=[pb[j]], writes=[byt])
                        act.op(lambda e, j=j, dt_=dt_: e.activation(out=ysq[j][:], in_=ps[j][:, :], func=AF.Square), reads=[pb[j]], writes=[bysq[j]])
                        pe.op(lambda e, j=j, dt_=dt_: e.matmul(ps[4][:, :], lhsT=onesb, rhs=ysq[j][:], start=(dt_ == 0), stop=(dt_ == 7)), reads=[bysq[j], b_cst], writes=[pb[4]])
                    act.op(lambda e: e.activation(out=r2[:], in_=ps[4][:, :], func=AF.Ln, bias=cc("epsk"), scale=1.0), reads=[pb[4], b_cst], writes=[br2])
                    act.op(lambda e: e.activation(out=r2[:], in_=r2[:], func=AF.Exp, scale=-0.5), reads=[br2], writes=[br2])
                    for k in range(8):
                        dve.op(lambda e, k=k: e.scalar_tensor_tensor(out=yt[:, k, :], in0=yt[:, k, :], scalar=g32[:, 8 + k:9 + k], in1=r2[:], op0=ALU.mult, op1=ALU.mult),
                               reads=[byt, br2, b_vec], writes=[byt])
                        sp.dma(xg[k % 2][:], xT_own[k * 128:(k + 1) * 128, tg0:tg0 + 512], dxg[k % 2], writes=[bxg[k % 2]])
                        pool.op(lambda e, k=k: e.tensor_tensor(out=yt[:, k, :], in0=yt[:, k, :], in1=xg[k % 2][:], op=ALU.add), reads=[byt, bxg[k % 2]], writes=[byt])
                        pool.op(lambda e, k=k: e.tensor_copy(out=mgb[:, k, :], in_=yt[:, k, :]), reads=[byt], writes=[bmgb])
                    wp, wpb = load_w(lambda k: w_pg[k * 128:(k + 1) * 128, :], 8, 1024)
                    wl, wlb = load_w(lambda k: w_ple[k * 128:(k + 1) * 128, :], 2, 1024)
                    for dt_ in range(8):
                        j = dt_ % 2
                        proj_fm(wp, wpb, dt_ * 128, 128, mgb, bmgb, 512, j)
                        for kk in range(2):
                            pe.op(lambda e, kk=kk, j=j, dt_=dt_: e.matmul(ps[2 + j][:, :], lhsT=wl[:, kk, dt_ * 128:(dt_ + 1) * 128], rhs=pTb[:, kk, :], start=(kk == 0), stop=(kk == 1)),
                                  reads=[wlb, bpT], writes=[pb[2 + j]])
                        act.op(lambda e, j=j: e.activation(out=gt[j][:], in_=ps[j][:, :], func=AF.Sigmoid), reads=[pb[j]], writes=[bgt[j]])
                        dve.op(lambda e, j=j: e.tensor_tensor(out=tt[j][:], in0=ps[2 + j][:, :], in1=gt[j][:], op=ALU.mult), reads=[pb[2 + j], bgt[j]], writes=[btt[j]])
                        pool.op(lambda e, j=j, dt_=dt_: e.tensor_tensor(out=mg[:, dt_, :], in0=yt[:, dt_, :], in1=tt[j][:], op=ALU.add), reads=[btt[j], byt], writes=[bmg_])
                    for k in range(8):
                        sp.dma(out[k * 128:(k + 1) * 128, tg0:tg0 + 512], mg[:, k, :], osem2, reads=[bmg_] if k == 0 else [])
                    tk = (osem2.h, osem2.cnt, None)
                    bmg_.r[osem2] = tk
                    K.barrier()
        K.mute = False
        K.barrier()
    return nc


_NC_CACHE = {}


def _layer_inputs(layer, core, xfull_T, x_rows, inp, consts_np):
    blocks = [8 * l + core for l in range(LB)]
    own_idx = np.concatenate([np.arange(b * 128, (b + 1) * 128) for b in blocks])
    x_own = x_rows[own_idx]
    halo = np.zeros((32, D), np.float32)
    for l, b in enumerate(blocks):
        if b > 0:
            halo[2 * l:2 * l + 2] = x_rows[b * 128 - 2:b * 128]
    pos = np.asarray(inp["positions"])[0].astype(np.int32)
    cm, ms = make_masks(core)
    vecs = np.zeros((128, 64), np.float32)
    vecs[:, 0:8] = np.asarray(inp["g_pre"])[layer].reshape(8, 128).T
    vecs[:, 8:16] = np.asarray(inp["g_post"])[layer].reshape(8, 128).T
    vecs[:, 16:48] = np.asarray(inp["b_merge"])[layer].reshape(32, 128).T
    cwl = np.asarray(inp["conv_w"])[layer]
    vecs[:, 48:54] = cwl.reshape(3, 2, 128).transpose(2, 1, 0).reshape(128, 6)
    vecs[:, 54:56] = np.asarray(inp["conv_b"])[layer].reshape(2, 128).T
    lnb = np.concatenate([np.broadcast_to(np.asarray(inp["ln_g"])[layer][None, :], (128, 256)),
                          np.broadcast_to(np.asarray(inp["ln_b"])[layer][None, :], (128, 256))], axis=1)
    wsT = np.asarray(inp["w_spatial"])[layer].transpose(2, 0, 1).reshape(128, 512)
    return {
        "xT_full": xfull_T,
        "xT_own": np.ascontiguousarray(x_own.T),
        "xT_halo": np.ascontiguousarray(halo.T),
        "pT_own": np.ascontiguousarray(np.asarray(inp["p"])[layer, 0][own_idx].T),
        "pos_full": np.ascontiguousarray(pos[None, :]),
        "pos_own": np.ascontiguousarray(pos[own_idx][None, :]),
        "w_ext": inp["_w_ext"][layer],
        "w_branch": np.ascontiguousarray(np.asarray(inp["w_branch"])[layer]),
        "w_out": np.ascontiguousarray(np.asarray(inp["w_out"])[layer]),
        "w_ple": np.ascontiguousarray(np.asarray(inp["w_ple"])[layer]),
        "w_pg": np.ascontiguousarray(np.asarray(inp["w_ple_gate"])[layer]),
        "wsT": np.ascontiguousarray(wsT),
        "vecs": vecs,
        "lnb": np.ascontiguousarray(lnb.astype(np.float32)),
        "bsp": np.ascontiguousarray(np.asarray(inp["b_spatial"])[layer]),
        "consts": consts_np,
        "cm": cm, "ms": ms,
    }


def kernel(**inputs):
    inp = dict(inputs)
    w_in = np.asarray(inp["w_in"])
    inp["_w_ext"] = [np.ascontiguousarray(w_in[l][:, ALL_COLS]) for l in range(w_in.shape[0])]
    consts_np = make_consts()
    if "nc" not in _NC_CACHE:
        _NC_CACHE["nc"] = build_program()
    nc = _NC_CACHE["nc"]
    x_rows = np.asarray(inp["x"])[0].astype(np.float32)
    depth = w_in.shape[0]
    for layer in range(depth):
        xfull_T = np.ascontiguousarray(x_rows.T)
        in_maps = [_layer_inputs(layer, c, xfull_T, x_rows, inp, consts_np) for c in range(NCORES)]
        res = run_bass_kernel_spmd(nc, in_maps, core_ids=list(range(NCORES)))
        x_new = np.empty_like(x_rows)
        for c in range(NCORES):
            o = np.asarray(res.results[c]["xT_next"]).T
            for l in range(LB):
                b = 8 * l + c
                x_new[b * 128:(b + 1) * 128] = o[l * 128:(l + 1) * 128]
        x_rows = x_new
    return x_rows[None].astype(np.float32)
```
